# Optimizing a Trainium2 kernel written in Bass

```python
import math
import jax, jax.numpy as jnp
from jax import lax
import numpy as np

D_MODEL = 1024
BATCH = 2
SEQ = 8192
DEPTH = 2

CHUNK = 64
Q_BLOCK = 128
N_META = 16
DIFF_HEADS = 4
DIFF_QK_DIM = 64
DIFF_V_DIM = 2 * DIFF_QK_DIM
FOX_HEADS = 4
FOX_HEAD_DIM = 64
FOX_FORGET_BIAS = 2.0
GDN_HEADS = 4
GDN_HEAD_DIM = 64
CONV_K = 4
N_EXPERTS = 32
TOP_K = 4
D_EXPERT = D_MODEL
MOE_BLOCK = 128
SWIGLU_LIMIT = 7.0
SWIGLU_ALPHA = 1.702
DEEPNORM_ALPHA = (2 * DEPTH) ** 0.25
DEEPNORM_BETA = (8 * DEPTH) ** -0.25
MASK_VALUE = -1e30

DIFF_QK_WIDTH = DIFF_HEADS * 2 * DIFF_QK_DIM
DIFF_V_WIDTH = DIFF_HEADS * DIFF_V_DIM
FOX_WIDTH = FOX_HEADS * FOX_HEAD_DIM
GDN_WIDTH = GDN_HEADS * GDN_HEAD_DIM
GDN_CONV_CH = 3 * GDN_WIDTH
MIX_WIDTH = DIFF_V_WIDTH + FOX_WIDTH + GDN_WIDTH
IN_PROJ_SIZES = (DIFF_QK_WIDTH, DIFF_QK_WIDTH, DIFF_V_WIDTH, FOX_WIDTH, FOX_WIDTH, FOX_WIDTH, FOX_HEADS,
                 GDN_CONV_CH, GDN_HEADS, GDN_HEADS, GDN_WIDTH)
N_PROJ = 3340

kernel_name = 'hybrid_diff_fox_gdn_moe_deepnorm'


def _split_points():
    return [int(s) for s in np.cumsum(IN_PROJ_SIZES)[:-1]]


def _in_proj_column_scale():
    scale = np.ones((N_PROJ,), np.float32)
    offs = np.concatenate([[0], np.cumsum(IN_PROJ_SIZES)])
    for seg in (2, 5):
        scale[offs[seg]:offs[seg + 1]] = DEEPNORM_BETA
    scale[offs[7] + 2 * GDN_WIDTH:offs[8]] = DEEPNORM_BETA
    return scale


def _diff_lambda_init(layer):
    return 0.8 - 0.6 * math.exp(-0.3 * layer)


def _layer_norm(x, g, b, eps=1e-5):
    xf = x.astype(jnp.float32)
    mu = jnp.mean(xf, -1, keepdims=True)
    var = jnp.mean(jnp.square(xf - mu), -1, keepdims=True)
    return ((xf - mu) * lax.rsqrt(var + eps) * g + b).astype(x.dtype)


def _rms_norm(x, g, eps):
    xf = x.astype(jnp.float32)
    return (xf * lax.rsqrt(jnp.mean(jnp.square(xf), -1, keepdims=True) + eps) * g).astype(x.dtype)


def _l2_normalize(x, eps=1e-6):
    return x * lax.rsqrt(jnp.sum(jnp.square(x), -1, keepdims=True) + eps)


def _sweep_query_blocks(block_fn, seq_len):
    out = lax.map(block_fn, jnp.arange(seq_len // Q_BLOCK))
    nb, b, h, qb, dv = out.shape
    return out.transpose(1, 2, 0, 3, 4).reshape(b, h, nb * qb, dv)


def _differential_attention(q, k, v, lam_vecs, subln_g, lambda_init, key_valid):
    b, l, _ = q.shape
    q = q.reshape(b, l, DIFF_HEADS, 2, DIFF_QK_DIM).transpose(3, 0, 2, 1, 4)
    k = k.reshape(b, l, DIFF_HEADS, 2, DIFF_QK_DIM).transpose(3, 0, 2, 1, 4)
    v = v.reshape(b, l, DIFF_HEADS, DIFF_V_DIM).transpose(0, 2, 1, 3)
    lv = lam_vecs.astype(jnp.float32)
    lam = jnp.exp(jnp.sum(lv[0] * lv[1])) - jnp.exp(jnp.sum(lv[2] * lv[3])) + lambda_init
    key_chunk = jnp.arange(l) // CHUNK
    scale = DIFF_QK_DIM ** -0.5

    def block(i):
        qi = lax.dynamic_slice_in_dim(q, i * Q_BLOCK, Q_BLOCK, axis=3)
        q_chunk = (i * Q_BLOCK + jnp.arange(Q_BLOCK)) // CHUNK
        visible = (key_chunk[None, :] <= q_chunk[:, None]) & key_valid[None, :]
        s = jnp.einsum('mbhqd,mbhkd->mbhqk', qi, k).astype(jnp.float32) * scale
        p = jax.nn.softmax(jnp.where(visible, s, MASK_VALUE), axis=-1)
        p = p[0] - lam * p[1]
        return jnp.einsum('bhqk,bhkd->bhqd', p.astype(v.dtype), v)

    o = _sweep_query_blocks(block, l)
    o = _rms_norm(o, subln_g, 1e-5) * (1.0 - lambda_init)
    return o.transpose(0, 2, 1, 3).reshape(b, l, DIFF_V_WIDTH)


def _forgetting_attention(q, k, v, f_logit, f_bias, key_valid):
    b, l, _ = q.shape

    def heads(t):
        return t.reshape(b, l, FOX_HEADS, FOX_HEAD_DIM).transpose(0, 2, 1, 3)

    q, k, v = heads(q), heads(k), heads(v)
    log_f = jax.nn.log_sigmoid((f_logit + f_bias).astype(jnp.float32))
    cum = jnp.cumsum(log_f, axis=1).transpose(0, 2, 1)
    key_pos = jnp.arange(l)
    scale = FOX_HEAD_DIM ** -0.5

    def block(i):
        start = i * Q_BLOCK
        qi = lax.dynamic_slice_in_dim(q, start, Q_BLOCK, axis=2)
        ci = lax.dynamic_slice_in_dim(cum, start, Q_BLOCK, axis=2)
        q_pos = start + jnp.arange(Q_BLOCK)
        visible = (key_pos[None, :] <= q_pos[:, None]) & key_valid[None, :]
        s = (jnp.einsum('bhqd,bhkd->bhqk', qi, k).astype(jnp.float32) * scale
             + ci[..., :, None] - cum[..., None, :])
        p = jax.nn.softmax(jnp.where(visible, s, MASK_VALUE), axis=-1)
        return jnp.einsum('bhqk,bhkd->bhqd', p.astype(v.dtype), v)

    o = _sweep_query_blocks(block, l)
    return o.transpose(0, 2, 1, 3).reshape(b, l, FOX_WIDTH)


def _causal_depthwise_conv(x, w):
    return lax.conv_general_dilated(x, w[:, None, :], window_strides=(1,), padding=[(CONV_K - 1, 0)],
                                    dimension_numbers=('NWC', 'WIO', 'NWC'),
                                    feature_group_count=x.shape[-1])


def _unit_lower_solve(a, rhs):
    return lax.linalg.triangular_solve(a, rhs, left_side=True, lower=True, unit_diagonal=True)


def _chunk_gated_delta_rule(q, k, v, beta, g):
    b, l, h, dk = q.shape
    dv = v.shape[-1]
    n = l // CHUNK

    def chunks(t):
        return t.reshape(b, n, CHUNK, h, -1).transpose(0, 3, 1, 2, 4)

    q, k, v = chunks(q), chunks(k), chunks(v)
    beta = beta.reshape(b, n, CHUNK, h).transpose(0, 3, 1, 2)
    gc = jnp.cumsum(g.reshape(b, n, CHUNK, h).transpose(0, 3, 1, 2), axis=-1)
    causal = jnp.tril(jnp.ones((CHUNK, CHUNK), bool))
    strict = jnp.tril(jnp.ones((CHUNK, CHUNK), bool), -1)
    decay = jnp.exp(jnp.where(causal, gc[..., :, None] - gc[..., None, :], -jnp.inf))
    a = jnp.where(strict, jnp.einsum('bhncd,bhnsd->bhncs', k, k) * beta[..., :, None] * decay, 0.0)
    a = a + jnp.eye(CHUNK, dtype=a.dtype)
    u_base = _unit_lower_solve(a, v * beta[..., None])
    w_dec = _unit_lower_solve(a, k * (beta * jnp.exp(gc))[..., None])
    qk = jnp.einsum('bhncd,bhnsd->bhncs', q, k) * decay
    q_dec = q * jnp.exp(gc)[..., None]
    k_dec = k * jnp.exp(gc[..., -1:] - gc)[..., None]
    chunk_decay = jnp.exp(gc[..., -1])

    def step(state, xs):
        u_n, w_n, qk_n, qd_n, kd_n, cd_n = xs
        v_new = u_n - jnp.einsum('bhcd,bhdv->bhcv', w_n, state)
        o_n = jnp.einsum('bhcd,bhdv->bhcv', qd_n, state) + jnp.einsum('bhcs,bhsv->bhcv', qk_n, v_new)
        state = state * cd_n[..., None, None] + jnp.einsum('bhcd,bhcv->bhdv', kd_n, v_new)
        return state, o_n

    xs = tuple(jnp.moveaxis(t, 2, 0) for t in (u_base, w_dec, qk, q_dec, k_dec, chunk_decay))
    state0 = jnp.zeros((b, h, dk, dv), jnp.float32)
    _, o = lax.scan(step, state0, xs)
    return o.transpose(1, 0, 3, 2, 4).reshape(b, l, h, dv)


def _gated_deltanet(qkv, b_logit, a_logit, z, conv_w, a_log, dt_bias, norm_g):
    b, l, _ = qkv.shape
    qkv = jax.nn.silu(_causal_depthwise_conv(qkv, conv_w))
    q, k, v = jnp.split(qkv.astype(jnp.float32), 3, axis=-1)

    def heads(t):
        return t.reshape(b, l, GDN_HEADS, GDN_HEAD_DIM)

    q = _l2_normalize(heads(q)) * GDN_HEAD_DIM ** -0.5
    k = _l2_normalize(heads(k))
    beta = jax.nn.sigmoid(b_logit.astype(jnp.float32))
    g = -jnp.exp(a_log.astype(jnp.float32)) * jax.nn.softplus(a_logit.astype(jnp.float32) + dt_bias)
    o = _chunk_gated_delta_rule(q, k, heads(v), beta, g).astype(z.dtype)
    o = _rms_norm(o, norm_g, 1e-6) * jax.nn.silu(heads(z))
    return o.reshape(b, l, GDN_WIDTH)


def _hybrid_mixer(h, w_in, diff_lambda, diff_subln_g, fox_forget_b, gdn_conv_w, gdn_a_log, gdn_dt_bias,
                  gdn_norm_g, w_out, lambda_init, key_valid):
    proj = h @ w_in
    dq, dk, dv, fq, fk, fv, ff, gqkv, gb, ga, gz = jnp.split(proj, _split_points(), axis=-1)
    y_diff = _differential_attention(dq, dk, dv, diff_lambda, diff_subln_g, lambda_init, key_valid)
    y_fox = _forgetting_attention(fq, fk, fv, ff, fox_forget_b, key_valid)
    y_gdn = _gated_deltanet(gqkv, gb, ga, gz, gdn_conv_w, gdn_a_log, gdn_dt_bias, gdn_norm_g)
    return jnp.concatenate([y_diff, y_fox, y_gdn], axis=-1) @ w_out


def _moe_ffn(h, router_w, router_b, w1, b1, w2, b2):
    b, l, d = h.shape
    xt = h.reshape(-1, d)
    t = xt.shape[0]
    logits = (xt @ router_w + router_b).astype(jnp.float32)
    top_val, top_idx = lax.top_k(logits, TOP_K)
    gate = jax.nn.softmax(top_val, axis=-1)
    e_flat = top_idx.reshape(-1)
    tok_flat = jnp.arange(t * TOP_K) // TOP_K
    order = jnp.argsort(e_flat)
    e_sorted = e_flat[order]
    counts = jnp.bincount(e_flat, length=N_EXPERTS)
    padded = (counts + MOE_BLOCK - 1) // MOE_BLOCK * MOE_BLOCK
    pad_end = jnp.cumsum(padded)
    pad_start = pad_end - padded
    start = jnp.cumsum(counts) - counts
    dest = pad_start[e_sorted] + jnp.arange(t * TOP_K) - start[e_sorted]
    n_slots = -(-(t * TOP_K + N_EXPERTS * (MOE_BLOCK - 1)) // MOE_BLOCK) * MOE_BLOCK
    n_blocks = n_slots // MOE_BLOCK
    slot_tok = jnp.full((n_slots,), t, jnp.int32).at[dest].set(tok_flat[order])
    slot_gate = jnp.zeros((n_slots,), jnp.float32).at[dest].set(gate.reshape(-1)[order])
    block_expert = jnp.minimum(jnp.searchsorted(pad_end, jnp.arange(n_blocks) * MOE_BLOCK, side='right'),
                               N_EXPERTS - 1)
    x_pad = jnp.concatenate([xt, jnp.zeros((1, d), xt.dtype)], axis=0)
    xs = x_pad[slot_tok].reshape(n_blocks, MOE_BLOCK, d)

    def expert_block(args):
        xb, e = args
        hid = xb @ w1[e] + b1[e]
        glu, lin = jnp.split(hid, 2, axis=-1)
        glu = jnp.minimum(glu, SWIGLU_LIMIT)
        lin = jnp.clip(lin, -SWIGLU_LIMIT, SWIGLU_LIMIT)
        act = glu * jax.nn.sigmoid(SWIGLU_ALPHA * glu) * (lin + 1.0)
        return act @ w2[e] + b2[e]

    ys = lax.map(expert_block, (xs, block_expert)).reshape(n_slots, d)
    out = jnp.zeros((t + 1, d), ys.dtype).at[slot_tok].add(ys * slot_gate[:, None].astype(ys.dtype))
    return out[:t].reshape(b, l, d)


def setup_inputs(seed: int = 0) -> dict:
    key = jax.random.key(seed)
    ks = jax.random.split(key, 23)
    f32 = jnp.float32

    def normal(k, shape, scale):
        return jax.random.normal(k, shape, f32) * scale

    x = normal(ks[0], (BATCH, SEQ, D_MODEL), 1.0)
    meta_tokens = normal(ks[1], (N_META, D_MODEL), 1.0)
    ln_in_g = 1.0 + normal(ks[2], (D_MODEL,), 0.02)
    ln_in_b = normal(ks[3], (D_MODEL,), 0.02)
    w_in = normal(ks[4], (DEPTH, D_MODEL, N_PROJ), D_MODEL ** -0.5) * jnp.asarray(_in_proj_column_scale())
    diff_lambda = normal(ks[5], (DEPTH, 4, DIFF_QK_DIM), 0.1)
    diff_subln_g = 1.0 + normal(ks[6], (DEPTH, DIFF_V_DIM), 0.02)
    fox_forget_b = FOX_FORGET_BIAS + normal(ks[7], (DEPTH, FOX_HEADS), 0.1)
    gdn_conv_w = normal(ks[8], (DEPTH, CONV_K, GDN_CONV_CH), CONV_K ** -0.5)
    gdn_a_log = jnp.log(jax.random.uniform(ks[9], (DEPTH, GDN_HEADS), f32, 1.0, 16.0))
    dt = jnp.exp(jax.random.uniform(ks[10], (DEPTH, GDN_HEADS), f32, math.log(1e-3), math.log(0.1)))
    gdn_dt_bias = dt + jnp.log(-jnp.expm1(-dt))
    gdn_norm_g = 1.0 + normal(ks[11], (DEPTH, GDN_HEAD_DIM), 0.02)
    w_out = normal(ks[12], (DEPTH, MIX_WIDTH, D_MODEL), MIX_WIDTH ** -0.5 * DEEPNORM_BETA)
    ln1_g = 1.0 + normal(ks[13], (DEPTH, D_MODEL), 0.02)
    ln1_b = normal(ks[14], (DEPTH, D_MODEL), 0.02)
    router_w = normal(ks[15], (DEPTH, D_MODEL, N_EXPERTS), D_MODEL ** -0.5)
    router_b = normal(ks[16], (DEPTH, N_EXPERTS), 0.01)
    expert_w1 = normal(ks[17], (DEPTH, N_EXPERTS, D_MODEL, 2 * D_EXPERT), D_MODEL ** -0.5 * DEEPNORM_BETA)
    expert_b1 = normal(ks[18], (DEPTH, N_EXPERTS, 2 * D_EXPERT), 0.02)
    expert_w2 = normal(ks[19], (DEPTH, N_EXPERTS, D_EXPERT, D_MODEL), D_EXPERT ** -0.5 * DEEPNORM_BETA)
    expert_b2 = normal(ks[20], (DEPTH, N_EXPERTS, D_MODEL), 0.02)
    ln2_g = 1.0 + normal(ks[21], (DEPTH, D_MODEL), 0.02)
    ln2_b = normal(ks[22], (DEPTH, D_MODEL), 0.02)
    return {'x': x, 'meta_tokens': meta_tokens, 'ln_in_g': ln_in_g, 'ln_in_b': ln_in_b, 'w_in': w_in,
            'diff_lambda': diff_lambda, 'diff_subln_g': diff_subln_g, 'fox_forget_b': fox_forget_b,
            'gdn_conv_w': gdn_conv_w, 'gdn_a_log': gdn_a_log, 'gdn_dt_bias': gdn_dt_bias,
            'gdn_norm_g': gdn_norm_g, 'w_out': w_out, 'ln1_g': ln1_g, 'ln1_b': ln1_b,
            'router_w': router_w, 'router_b': router_b, 'expert_w1': expert_w1, 'expert_b1': expert_b1,
            'expert_w2': expert_w2, 'expert_b2': expert_b2, 'ln2_g': ln2_g, 'ln2_b': ln2_b}


def reference(x, meta_tokens, ln_in_g, ln_in_b, w_in, diff_lambda, diff_subln_g, fox_forget_b, gdn_conv_w,
              gdn_a_log, gdn_dt_bias, gdn_norm_g, w_out, ln1_g, ln1_b, router_w, router_b, expert_w1,
              expert_b1, expert_w2, expert_b2, ln2_g, ln2_b):
    b, s, d = x.shape
    n_lead = (-(s + N_META)) % Q_BLOCK
    l = n_lead + N_META + s
    stream = jnp.concatenate([jnp.zeros((b, n_lead, d), x.dtype),
                              jnp.broadcast_to(meta_tokens.astype(x.dtype)[None], (b, N_META, d)),
                              x], axis=1)
    stream = _layer_norm(stream, ln_in_g, ln_in_b)
    key_valid = jnp.arange(l) >= n_lead
    in_mask = key_valid[None, :, None].astype(x.dtype)
    for layer in range(DEPTH):
        mix = _hybrid_mixer(stream * in_mask, w_in[layer], diff_lambda[layer], diff_subln_g[layer],
                            fox_forget_b[layer], gdn_conv_w[layer], gdn_a_log[layer], gdn_dt_bias[layer],
                            gdn_norm_g[layer], w_out[layer], _diff_lambda_init(layer), key_valid)
        stream = _layer_norm(DEEPNORM_ALPHA * stream + mix, ln1_g[layer], ln1_b[layer])
        ffn = _moe_ffn(stream, router_w[layer], router_b[layer], expert_w1[layer], expert_b1[layer],
                       expert_w2[layer], expert_b2[layer])
        stream = _layer_norm(DEEPNORM_ALPHA * stream + ffn, ln2_g[layer], ln2_b[layer])
    return stream[:, n_lead + N_META:]
```

```python
import contextlib
import os
import numpy as np
import ml_dtypes
import concourse.bass as bass
import concourse.mybir as mybir
from concourse.bass_utils import run_bass_kernel_spmd

F32 = mybir.dt.float32
BF16 = mybir.dt.bfloat16
AF = mybir.ActivationFunctionType
ALU = mybir.AluOpType
AX = mybir.AxisListType

D = 1024
L = 8320
NTL = 65
NTB = 17
ALPHA = 4 ** 0.25
TILE_START = [0, 17, 33, 49, 65]


class Res:
    __slots__ = ("w", "rs")

    def __init__(self):
        self.w = None
        self.rs = {}


class Sched:
    def __init__(self, nc, es, nds=14):
        self.nc = nc
        self.E = {"pe": nc.tensor, "act": nc.scalar, "dve": nc.vector, "pool": nc.gpsimd, "sp": nc.sync}
        self.sem = {k: es.enter_context(nc.semaphore("c_" + k)) for k in ("pe", "act", "dve", "pool")}
        self.cnt = {k: 0 for k in self.sem}
        self.seen = {k: {} for k in self.E}
        self.dsem = [es.enter_context(nc.semaphore("d%d" % i)) for i in range(nds)]
        self.dcnt = [0] * nds
        self.dnext = 0
        self.out_events = []

    def _sem(self, key):
        return self.dsem[key[1]] if isinstance(key, tuple) else self.sem[key]

    def _wait(self, e, ev):
        key, val = ev
        if e == "pe" and key == "pe":
            return
        if self.seen[e].get(key, 0) >= val:
            return
        self.E[e].wait_ge(self._sem(key), val)
        self.seen[e][key] = val

    def _deps(self, e, r, w):
        evs = {}
        for x in r:
            if x.w is not None:
                evs[x.w[0]] = max(evs.get(x.w[0], 0), x.w[1])
        for x in w:
            if x.w is not None:
                evs[x.w[0]] = max(evs.get(x.w[0], 0), x.w[1])
            for k, v in x.rs.items():
                evs[k] = max(evs.get(k, 0), v)
        for k, v in evs.items():
            self._wait(e, (k, v))

    def _post(self, ev, r, w):
        for x in r:
            x.rs[ev[0]] = max(x.rs.get(ev[0], 0), ev[1])
        for x in w:
            x.w = ev
            x.rs = {}

    def op(self, e, fn, r=(), w=()):
        self._deps(e, r, w)
        ins = fn(self.E[e])
        self.cnt[e] += 1
        ins.then_inc(self.sem[e], 1)
        ev = (e, self.cnt[e])
        self._post(ev, r, w)
        return ev

    def dma(self, e, out, in_, r=(), w=(), is_out=False, **kw):
        self._deps(e, r, w)
        i = self.dnext
        self.dnext = (self.dnext + 1) % len(self.dsem)
        key = ("d", i)
        if self.dcnt[i] > 0:
            self._wait(e, (key, 16 * self.dcnt[i]))
        ins = self.E[e].dma_start(out=out, in_=in_, **kw)
        self.dcnt[i] += 1
        ins.then_inc(self.dsem[i], 16)
        ev = (key, 16 * self.dcnt[i])
        self._post(ev, r, w)
        if is_out:
            self.out_events.append(ev)
        return ev

    def barrier(self):
        for e in self.E:
            for k in self.sem:
                if self.cnt[k] > 0:
                    self._wait(e, (k, self.cnt[k])) if not (e == k) else self._wait_self(e)
            for i in range(len(self.dsem)):
                if self.dcnt[i] > 0:
                    self._wait(e, (("d", i), 16 * self.dcnt[i]))

    def _wait_self(self, e):
        if self.seen[e].get(e, 0) < self.cnt[e]:
            self.E[e].wait_ge(self.sem[e], self.cnt[e])
            self.seen[e][e] = self.cnt[e]

    def finish(self):
        evs = {}
        for k, v in self.out_events:
            evs[k] = max(evs.get(k, 0), v)
        for k, v in evs.items():
            self._wait("sp", (k, v))


def _layernorm_tile(S, es_tiles, src, dst, g_bc, b_bc, r_src, w_dst, rg):
    st, mv, rstd = es_tiles["st"], es_tiles["mv"], es_tiles["rstd"]
    rs = es_tiles["res"]
    for h in range(2):
        S.op("dve", lambda E, h=h: E.bn_stats(out=st[:, h, :], in_=src[:, h * 512:(h + 1) * 512]), r=r_src, w=[rs])
    S.op("dve", lambda E: E.bn_aggr(out=mv[:], in_=st[:].rearrange("p a b -> p (a b)")), r=[rs], w=[rs])
    S.op("act", lambda E: E.activation(out=rstd[:], in_=mv[:, 1:2], func=AF.Sqrt, bias=es_tiles["eps"][:], scale=1.0), r=[rs, es_tiles["eps_r"]], w=[rs])
    S.op("dve", lambda E: E.reciprocal(out=rstd[:], in_=rstd[:]), r=[rs], w=[rs])
    S.op("dve", lambda E: E.tensor_scalar(out=dst, in0=src, scalar1=mv[:, 0:1], scalar2=rstd[:], op0=ALU.subtract, op1=ALU.mult), r=list(r_src) + [rs], w=w_dst)
    S.op("dve", lambda E: E.tensor_tensor(out=dst, in0=dst, in1=g_bc[:], op=ALU.mult), r=[rg], w=w_dst)
    S.op("dve", lambda E: E.tensor_tensor(out=dst, in0=dst, in1=b_bc[:], op=ALU.add), r=[rg], w=w_dst)


def build_B(ne=32, debug=False):
    nc = bass.Bass("TRN2", target_bir_lowering=False)
    es = contextlib.ExitStack()
    NTOK = NTB * 128

    def din(name, shape, dt=F32):
        return nc.dram_tensor(name, shape, dt, kind="ExternalInput").ap()

    yg = din("yg", [NTOK, D], BF16)
    s32 = din("s32", [NTOK, D])
    wout = din("wout", [D, D])
    ln1g, ln1b, ln2g, ln2b = din("ln1g", [1, D]), din("ln1b", [1, D]), din("ln2g", [1, D]), din("ln2b", [1, D])
    rw = din("rw", [D, 32])
    rbias = din("rb", [1, 32])
    w1 = din("w1", [max(ne, 1), D, 2 * D])
    b1 = din("b1", [32, 2 * D])
    w2 = din("w2", [max(ne, 1), D, D])
    b2 = din("b2", [32, D])
    out32 = nc.dram_tensor("out32", [NTOK, D], F32, kind="ExternalOutput").ap()
    outbf = nc.dram_tensor("outbf", [NTOK, D], BF16, kind="ExternalOutput").ap()

    if debug:
        dbg_acc = nc.dram_tensor("dbg_acc", [NTOK, D], F32, kind="ExternalOutput").ap()
        dbg_x1 = nc.dram_tensor("dbg_x1", [NTOK, D], F32, kind="ExternalOutput").ap()
        dbg_gates = nc.dram_tensor("dbg_gates", [NTOK, 32], F32, kind="ExternalOutput").ap()
        dbg_b1c = nc.dram_tensor("dbg_b1c", [128, 512], F32, kind="ExternalOutput").ap()
        dbg_act = nc.dram_tensor("dbg_act", [128, 8 * 512], BF16, kind="ExternalOutput").ap()
        dbg_gt = nc.dram_tensor("dbg_gt", [128, 3 * 512], F32, kind="ExternalOutput").ap()
    S = Sched(nc, es)
    sb = lambda name, shape, dt=F32: es.enter_context(nc.sbuf_tensor(name, shape, dt))

    acc = sb("acc", [128, NTB, D])
    xT = sb("xT", [128, 8, NTOK], BF16)
    gates = sb("gates", [128, NTB, 32])
    b1c = sb("b1c", [128, 32, 16])
    identb = sb("identb", [128, 128], BF16)
    identf = sb("identf", [128, 128])
    eps = sb("eps", [128, 1])
    st = sb("st", [128, 2, 6])
    mv = sb("mv", [128, 2])
    rstd = sb("rstd", [128, 1])
    r_acc = [Res() for _ in range(NTB)]
    r_xT = [Res() for _ in range(NTB)]
    r_gates = [Res() for _ in range(NTB)]
    r_b1c, r_id, r_eps, r_ln = Res(), Res(), Res(), Res()
    lnt = {"st": st, "mv": mv, "rstd": rstd, "res": r_ln, "eps": eps, "eps_r": r_eps}

    S.op("pool", lambda E: E.memset(identf[:], 1.0), w=[r_id])
    S.op("pool", lambda E: E.affine_select(out=identf[:], in_=identf[:], pattern=[[-1, 128]], compare_op=ALU.is_equal, fill=0.0, base=0, channel_multiplier=1), r=[r_id], w=[r_id])
    S.op("pool", lambda E: E.tensor_copy(out=identb[:], in_=identf[:]), r=[r_id], w=[r_id])
    S.op("pool", lambda E: E.memset(eps[:], 1e-5), w=[r_eps])

    with contextlib.ExitStack() as e1:
        sb1 = lambda name, shape, dt=F32: e1.enter_context(nc.sbuf_tensor("a1_" + name, shape, dt))
        ps1 = lambda name, shape, dt=F32: e1.enter_context(nc.psum_tensor("a1_" + name, shape, dt))
        woutb = sb1("woutb", [128, 8, D], BF16)
        g1, bb1 = sb1("g1", [128, D]), sb1("bb1", [128, D])
        rwf = sb1("rwf", [128, 8, 32])
        rbb = sb1("rbb", [128, 32])
        b2all = sb1("b2all", [32, D])
        ytile = [sb1("ytile%d" % i, [128, D], BF16) for i in range(2)]
        stile = [sb1("stile%d" % i, [128, D]) for i in range(2)]
        yT = sb1("yT", [128, 8, 128], BF16)
        x1 = sb1("x1", [128, D])
        xnb = sb1("xnb", [128, D], BF16)
        xTf = sb1("xTf", [128, 8, 128])
        lg = sb1("lg", [128, 32])
        m8 = sb1("m8", [128, 8])
        nmx = sb1("nmx", [128, 1])
        msk = sb1("msk", [128, 32])
        ex = sb1("ex", [128, 32])
        den = sb1("den", [128, 1])
        gT = sb1("gT", [32, 128])
        tp = ps1("tp", [128, 8, 128], BF16)
        mx = [ps1("mx%d" % i, [128, 512]) for i in range(2)]
        tpf = ps1("tpf", [128, 4, 128])
        lgp = ps1("lgp", [128, 32])
        gtp = ps1("gtp", [32, 128])
        bt = [ps1("bt%d" % i, [128, 512]) for i in range(2)]
        r_w, r_g1, r_rw, r_b2 = Res(), Res(), Res(), Res()
        r_yt, r_st = [Res(), Res()], [Res(), Res()]
        r_yT, r_x1, r_xnb, r_xTf, r_sm, r_gT = Res(), Res(), Res(), Res(), Res(), Res()
        r_tp, r_mx, r_tpf, r_lgp, r_gtp, r_bt = Res(), [Res(), Res()], Res(), Res(), Res(), [Res(), Res()]

        for c in range(8):
            S.dma("pool", woutb[:, c, :], wout[c * 128:(c + 1) * 128, :], w=[r_w])
        S.dma("sp", g1[:], ln1g.partition_broadcast(128), w=[r_g1])
        S.dma("sp", bb1[:], ln1b.partition_broadcast(128), w=[r_g1])
        S.dma("sp", rwf[:], rw.rearrange("(c p) e -> p c e", p=128), w=[r_rw])
        S.dma("sp", rbb[:], rbias.partition_broadcast(128), w=[r_rw])
        S.dma("sp", b2all[:], b2[:, :], w=[r_b2])

        b1raw = sb1("b1raw", [32, 2 * D])
        r_b1raw = Res()
        S.dma("sp", b1raw[:], b1[:, :], w=[r_b1raw])
        for h in range(16):
            S.op("pe", lambda E, h=h: E.transpose(out=lgp[:], in_=b1raw[:, h * 128:(h + 1) * 128], identity=identf[0:32, 0:32]), r=[r_b1raw, r_id], w=[r_lgp])
            S.op("act", lambda E, h=h: E.copy(out=b1c[:, :, h], in_=lgp[:]), r=[r_lgp], w=[r_b1c])

        for T in range(NTB):
            yt, st_ = ytile[T % 2], stile[T % 2]
            S.dma("sp", yt[:], yg[T * 128:(T + 1) * 128, :], w=[r_yt[T % 2]])
            S.dma("sp", st_[:], s32[T * 128:(T + 1) * 128, :], w=[r_st[T % 2]])
            for c in range(8):
                S.op("pe", lambda E, c=c: E.transpose(out=tp[:, c, :], in_=yt[:, c * 128:(c + 1) * 128], identity=identb[:]), r=[r_yt[T % 2], r_id], w=[r_tp])
            S.op("act", lambda E: E.copy(out=yT[:], in_=tp[:]), r=[r_tp], w=[r_yT])
            for h in range(2):
                for c in range(8):
                    S.op("pe", lambda E, h=h, c=c: E.matmul(mx[h][:], lhsT=yT[:, c, :], rhs=woutb[:, c, h * 512:(h + 1) * 512], start=(c == 0), stop=(c == 7)), r=[r_yT, r_w], w=[r_mx[h]])
                S.op("dve", lambda E, h=h: E.scalar_tensor_tensor(out=x1[:, h * 512:(h + 1) * 512], in0=st_[:, h * 512:(h + 1) * 512], scalar=ALPHA, in1=mx[h][:], op0=ALU.mult, op1=ALU.add), r=[r_st[T % 2], r_mx[h]], w=[r_x1])
            if debug:
                S.dma("sp", dbg_x1[T * 128:(T + 1) * 128, :], x1[:], r=[r_x1], is_out=True)
            _layernorm_tile(S, lnt, x1[:], acc[:, T, :], g1, bb1, [r_x1], [r_acc[T]], r_g1)
            S.op("act", lambda E: E.copy(out=xnb[:], in_=acc[:, T, :]), r=[r_acc[T]], w=[r_xnb])
            for c in range(8):
                S.op("pe", lambda E, c=c: E.transpose(out=tp[:, c, :], in_=xnb[:, c * 128:(c + 1) * 128], identity=identb[:]), r=[r_xnb, r_id], w=[r_tp])
            S.op("act", lambda E: E.copy(out=xT[:, :, T * 128:(T + 1) * 128], in_=tp[:]), r=[r_tp], w=[r_xT[T]])
            for q in range(2):
                for c in range(4):
                    S.op("pe", lambda E, q=q, c=c: E.transpose(out=tpf[:, c, :], in_=acc[:, T, (q * 4 + c) * 128:(q * 4 + c + 1) * 128], identity=identf[:]), r=[r_acc[T], r_id], w=[r_tpf])
                S.op("act", lambda E, q=q: E.copy(out=xTf[:, q * 4:(q + 1) * 4, :], in_=tpf[:]), r=[r_tpf], w=[r_xTf])
            for c in range(8):
                S.op("pe", lambda E, c=c: E.matmul(lgp[:], lhsT=xTf[:, c, :], rhs=rwf[:, c, :], start=(c == 0), stop=(c == 7)), r=[r_xTf, r_rw], w=[r_lgp])
            S.op("dve", lambda E: E.tensor_tensor(out=lg[:], in0=lgp[:], in1=rbb[:], op=ALU.add), r=[r_lgp, r_rw], w=[r_sm])
            S.op("dve", lambda E: E.max(out=m8[:], in_=lg[:]), r=[r_sm], w=[r_sm])
            S.op("dve", lambda E: E.tensor_scalar(out=nmx[:], in0=m8[:, 0:1], scalar1=-1.0, scalar2=None, op0=ALU.mult), r=[r_sm], w=[r_sm])
            S.op("dve", lambda E: E.tensor_scalar(out=msk[:], in0=lg[:], scalar1=m8[:, 3:4], scalar2=None, op0=ALU.is_ge), r=[r_sm], w=[r_sm])
            S.op("act", lambda E: E.activation(out=ex[:], in_=lg[:], func=AF.Exp, bias=nmx[:], scale=1.0), r=[r_sm], w=[r_sm])
            S.op("dve", lambda E: E.tensor_tensor(out=ex[:], in0=ex[:], in1=msk[:], op=ALU.mult), r=[r_sm], w=[r_sm])
            S.op("dve", lambda E: E.tensor_reduce(out=den[:], in_=ex[:], axis=AX.X, op=ALU.add), r=[r_sm], w=[r_sm])
            S.op("dve", lambda E: E.reciprocal(out=den[:], in_=den[:]), r=[r_sm], w=[r_sm])
            S.op("dve", lambda E: E.tensor_scalar(out=gates[:, T, :], in0=ex[:], scalar1=den[:], scalar2=None, op0=ALU.mult), r=[r_sm], w=[r_gates[T]])
            S.op("pe", lambda E: E.transpose(out=gtp[:], in_=gates[:, T, :], identity=identf[:]), r=[r_gates[T], r_id], w=[r_gtp])
            S.op("act", lambda E: E.copy(out=gT[:], in_=gtp[:]), r=[r_gtp], w=[r_gT])
            for h in range(2):
                S.op("pe", lambda E, h=h: E.matmul(bt[h][:], lhsT=gT[:], rhs=b2all[:, h * 512:(h + 1) * 512], start=True, stop=True), r=[r_gT, r_b2], w=[r_bt[h]])
                S.op("dve", lambda E, h=h: E.scalar_tensor_tensor(out=acc[:, T, h * 512:(h + 1) * 512], in0=acc[:, T, h * 512:(h + 1) * 512], scalar=ALPHA, in1=bt[h][:], op0=ALU.mult, op1=ALU.add), r=[r_bt[h], r_xnb, r_tpf], w=[r_acc[T]])

    S.barrier()
    if debug:
        for T in range(NTB):
            S.dma("sp", dbg_acc[T * 128:(T + 1) * 128, :], acc[:, T, :], r=[r_acc[T]], is_out=True)
            S.dma("sp", dbg_gates[T * 128:(T + 1) * 128, :], gates[:, T, :], r=[r_gates[T]], is_out=True)
    groups = [(g * 512, min(512, NTOK - g * 512)) for g in range((NTOK + 511) // 512)]
    with contextlib.ExitStack() as e2:
        sb2 = lambda name, shape, dt=F32: e2.enter_context(nc.sbuf_tensor("a2_" + name, shape, dt))
        ps2 = lambda name, shape, dt=F32: e2.enter_context(nc.psum_tensor("a2_" + name, shape, dt))
        w1b = [sb2("w1b%d" % i, [128, 8, 2 * D], BF16) for i in range(2)]
        w2b = sb2("w2b", [128, 8, D], BF16)
        actT = sb2("actT", [128, 8, 512], BF16)
        gt = sb2("gt", [128, 512])
        sg = sb2("sg", [128, 512])
        lt = sb2("lt", [128, 512])
        pg = [ps2("pg%d" % i, [128, 512]) for i in range(2)]
        pl = [ps2("pl%d" % i, [128, 512]) for i in range(2)]
        py = [ps2("py%d" % i, [128, 512]) for i in range(4)]
        r_w1 = [[Res() for _ in range(8)] for _ in range(2)]
        r_w2 = Res()
        r_actT = [Res() for _ in range(8)]
        r_gt, r_sg, r_lt = Res(), Res(), Res()
        r_pg, r_pl, r_py = [Res(), Res()], [Res(), Res()], [Res() for _ in range(4)]
        ipy = 0
        ih = 0

        def load_w1(e):
            for c in range(8):
                S.dma("pool", w1b[e % 2][:, c, :], w1[e, c * 128:(c + 1) * 128, :], w=[r_w1[e % 2][c]])

        if ne > 0:
            load_w1(0)
        for e in range(ne):
            for c in range(8):
                S.dma("pool", w2b[:, c, :], w2[e, c * 128:(c + 1) * 128, :], w=[r_w2])
            if e + 1 < ne:
                load_w1(e + 1)
            wb = w1b[e % 2]
            for (t0, N) in groups:
                for h in range(8):
                    k = ih % 2
                    ih += 1
                    for c in range(8):
                        S.op("pe", lambda E, c=c, h=h, k=k: E.matmul(pg[k][:, 0:N], lhsT=wb[:, c, h * 128:(h + 1) * 128], rhs=xT[:, c, t0:t0 + N], start=(c == 0), stop=(c == 7)), r=[r_w1[e % 2][c]] + r_xT[t0 // 128:(t0 + N) // 128], w=[r_pg[k]])
                    for c in range(8):
                        S.op("pe", lambda E, c=c, h=h, k=k: E.matmul(pl[k][:, 0:N], lhsT=wb[:, c, D + h * 128:D + (h + 1) * 128], rhs=xT[:, c, t0:t0 + N], start=(c == 0), stop=(c == 7)), r=[r_w1[e % 2][c]] + r_xT[t0 // 128:(t0 + N) // 128], w=[r_pl[k]])
                    S.op("dve", lambda E, h=h, k=k: E.tensor_scalar(out=gt[:, 0:N], in0=pg[k][:, 0:N], scalar1=b1c[:, e, h:h + 1], scalar2=7.0, op0=ALU.add, op1=ALU.min), r=[r_pg[k], r_b1c], w=[r_gt])
                    S.op("act", lambda E: E.activation(out=sg[:, 0:N], in_=gt[:, 0:N], func=AF.Sigmoid, scale=1.702), r=[r_gt], w=[r_sg])
                    S.op("dve", lambda E, h=h, k=k: E.tensor_scalar(out=lt[:, 0:N], in0=pl[k][:, 0:N], scalar1=b1c[:, e, 8 + h:9 + h], scalar2=7.0, op0=ALU.add, op1=ALU.min), r=[r_pl[k], r_b1c], w=[r_lt])
                    S.op("dve", lambda E: E.tensor_scalar(out=lt[:, 0:N], in0=lt[:, 0:N], scalar1=-7.0, scalar2=1.0, op0=ALU.max, op1=ALU.add), r=[r_lt], w=[r_lt])
                    S.op("dve", lambda E: E.tensor_tensor(out=gt[:, 0:N], in0=gt[:, 0:N], in1=sg[:, 0:N], op=ALU.mult), r=[r_sg], w=[r_gt])
                    S.op("dve", lambda E, h=h: E.tensor_tensor(out=actT[:, h, 0:N], in0=gt[:, 0:N], in1=lt[:, 0:N], op=ALU.mult), r=[r_gt, r_lt], w=[r_actT[h]])
                for s in range(N // 128):
                    T = t0 // 128 + s
                    for hf in range(2):
                        k = ipy % 4
                        ipy += 1
                        for h in range(8):
                            S.op("pe", lambda E, h=h, k=k, s=s, hf=hf: E.matmul(py[k][:], lhsT=actT[:, h, s * 128:(s + 1) * 128], rhs=w2b[:, h, hf * 512:(hf + 1) * 512], start=(h == 0), stop=(h == 7)), r=[r_actT[h], r_w2], w=[r_py[k]])
                        S.op("dve", lambda E, k=k, hf=hf, T=T: E.scalar_tensor_tensor(out=acc[:, T, hf * 512:(hf + 1) * 512], in0=py[k][:], scalar=gates[:, T, e:e + 1], in1=acc[:, T, hf * 512:(hf + 1) * 512], op0=ALU.mult, op1=ALU.add), r=[r_py[k], r_gates[T]], w=[r_acc[T]])

        if debug and ne > 0:
            S.dma("sp", dbg_b1c[:, :], b1c[:].rearrange("p a b -> p (a b)"), r=[r_b1c], is_out=True)
            S.dma("sp", dbg_act[:, :], actT[:].rearrange("p a b -> p (a b)"), r=r_actT, is_out=True)
            S.dma("sp", dbg_gt[:, 0:512], gt[:], r=[r_gt], is_out=True)
            S.dma("sp", dbg_gt[:, 512:1024], sg[:], r=[r_sg], is_out=True)
            S.dma("sp", dbg_gt[:, 1024:1536], lt[:], r=[r_lt], is_out=True)
    S.barrier()
    with contextlib.ExitStack() as e3:
        sb3 = lambda name, shape, dt=F32: e3.enter_context(nc.sbuf_tensor(name, shape, dt))
        g2, bb2 = sb3("g2", [128, D]), sb3("bb2", [128, D])
        ot = [sb3("ot%d" % i, [128, D]) for i in range(2)]
        ob = [sb3("ob%d" % i, [128, D], BF16) for i in range(2)]
        r_g2 = Res()
        r_ot, r_ob = [Res(), Res()], [Res(), Res()]
        S.dma("sp", g2[:], ln2g.partition_broadcast(128), w=[r_g2])
        S.dma("sp", bb2[:], ln2b.partition_broadcast(128), w=[r_g2])
        for T in range(NTB):
            k = T % 2
            _layernorm_tile(S, lnt, acc[:, T, :], ot[k][:], g2, bb2, [r_acc[T]], [r_ot[k]], r_g2)
            S.op("act", lambda E, k=k: E.copy(out=ob[k][:], in_=ot[k][:]), r=[r_ot[k]], w=[r_ob[k]])
            S.dma("sp", out32[T * 128:(T + 1) * 128, :], ot[k][:], r=[r_ot[k]], is_out=True)
            S.dma("sp", outbf[T * 128:(T + 1) * 128, :], ob[k][:], r=[r_ob[k]], is_out=True)
    S.finish()
    return nc


WC = 1024
C_DQ, C_DK, C_DV, C_FQ, C_FK, C_FV, C_GQ, C_GZ = 0, 128, 256, 384, 448, 576, 704, 896
P_LAM, P_DSG, P_FFB, P_ALOG, P_DTB, P_GNG, P_GCW, PRM = 0, 256, 384, 385, 386, 448, 512, 1280


def build_A(first, lam_init, stages=(1, 2, 3)):
    nc = bass.Bass("TRN2", target_bir_lowering=False)
    es = contextlib.ExitStack()

    def din(name, shape, dt=F32):
        return nc.dram_tensor(name, shape, dt, kind="ExternalInput").ap()

    if first:
        x = din("x", [8192, D])
        meta = din("meta", [16, D])
        lng, lnb = din("lng", [1, D]), din("lnb", [1, D])
        s32o = nc.dram_tensor("s32o", [L, D], F32, kind="ExternalOutput").ap()
    else:
        sbf = din("sbf", [L, D], BF16)
    w = din("w", [D, WC])
    prm = din("prm", [1, PRM])
    y = nc.dram_tensor("y", [L, 256], BF16, kind="ExternalOutput").ap()
    xTd = nc.dram_tensor("xTd", [8, 128, L], BF16).ap()

    S = Sched(nc, es)
    sb = lambda name, shape, dt=F32: es.enter_context(nc.sbuf_tensor(name, shape, dt))
    chunks = [(g * 512, min(512, L - g * 512)) for g in range((L + 511) // 512)]

    wbf = sb("wbf", [128, 8, WC], BF16)
    ybuf = sb("ybuf", [128, NTL, 256], BF16)
    identb = sb("identb", [128, 128], BF16)
    identf = sb("identf", [128, 128])
    trif = sb("trif", [128, 128])
    onesf = sb("onesf", [128, 128])
    cmaskb = sb("cmaskb", [128, 128], BF16)
    dmaskb = sb("dmaskb", [128, 128], BF16)
    smask = sb("smask", [128, 128])
    pmask = sb("pmask", [128, 1])
    c_eps5, c_eps6, c_one = sb("c_eps5", [128, 1]), sb("c_eps6", [128, 1]), sb("c_one", [128, 1])
    prow = sb("prow", [1, PRM])
    pcol = sb("pcol", [128, 8])
    r_w, r_y, r_c, r_xTd, r_prow, r_pcol = Res(), [Res() for _ in range(NTL)], Res(), Res(), Res(), Res()

    def aff(out, base, cm, step, op=ALU.is_ge):
        S.op("pool", lambda E: E.memset(out, 1.0), w=[r_c])
        S.op("pool", lambda E: E.affine_select(out=out, in_=out, pattern=[[step, 128]], compare_op=op, fill=0.0, base=base, channel_multiplier=cm), r=[r_c], w=[r_c])

    aff(identf[:], 0, 1, -1, ALU.is_equal)
    aff(trif[:], 0, -1, 1)
    aff(smask[:], -1, 1, -1)
    S.op("pool", lambda E: E.tensor_copy(out=identb[:], in_=identf[:]), r=[r_c], w=[r_c])
    S.op("pool", lambda E: E.tensor_copy(out=cmaskb[:], in_=trif[:]), r=[r_c], w=[r_c])
    S.op("pool", lambda E: E.memset(onesf[:], 1.0), w=[r_c])
    S.op("pool", lambda E: E.memset(dmaskb[:], 1.0), w=[r_c])
    S.op("pool", lambda E: E.memset(dmaskb[64:128, 0:64], 0.0), r=[r_c], w=[r_c])
    S.op("pool", lambda E: E.memset(pmask[:], 1.0), w=[r_c])
    S.op("pool", lambda E: E.memset(pmask[0:112, :], 0.0), r=[r_c], w=[r_c])
    S.op("pool", lambda E: E.memset(c_eps5[:], 1e-5), w=[r_c])
    S.op("pool", lambda E: E.memset(c_eps6[:], 1e-6), w=[r_c])
    S.op("pool", lambda E: E.memset(c_one[:], 1.0), w=[r_c])
    for c in range(8):
        S.dma("pool", wbf[:, c, :], w[c * 128:(c + 1) * 128, :], w=[r_w])
    S.dma("sp", prow[:], prm[:, :], w=[r_prow])

    with contextlib.ExitStack() as e0:
        sb0 = lambda name, shape, dt=F32: e0.enter_context(nc.sbuf_tensor(name, shape, dt))
        tp = e0.enter_context(nc.psum_tensor("tp0", [128, 8, 128], BF16))
        pbc = e0.enter_context(nc.psum_tensor("pbc", [128, 8]))
        r_tp, r_pbc = Res(), Res()
        xin = [sb0("xin%d" % i, [128, D], F32 if first else BF16) for i in range(2)]
        xb = [sb0("xb%d" % i, [128, D], BF16) for i in range(2)]
        xTt = [sb0("xTt%d" % i, [128, 8, 128], BF16) for i in range(2)]
        r_xin, r_xb, r_xTt = [Res(), Res()], [Res(), Res()], [Res(), Res()]
        srow = sb0("srow", [1, 8])
        tmp64 = sb0("tmp64", [1, 128])
        r_srow = Res()
        S.op("dve", lambda E: E.memset(srow[:], 0.0), w=[r_srow])
        S.op("dve", lambda E: E.tensor_tensor(out=tmp64[:, 0:64], in0=prow[:, P_LAM:P_LAM + 64], in1=prow[:, P_LAM + 64:P_LAM + 128], op=ALU.mult), r=[r_prow], w=[r_srow])
        S.op("dve", lambda E: E.tensor_tensor(out=tmp64[:, 64:128], in0=prow[:, P_LAM + 128:P_LAM + 192], in1=prow[:, P_LAM + 192:P_LAM + 256], op=ALU.mult), r=[r_prow], w=[r_srow])
        S.op("dve", lambda E: E.tensor_reduce(out=srow[:, 4:6], in_=tmp64[:].rearrange("p (a b) -> p a b", a=2), axis=AX.X, op=ALU.add), r=[r_srow], w=[r_srow])
        S.op("act", lambda E: E.activation(out=srow[:, 4:6], in_=srow[:, 4:6], func=AF.Exp), r=[r_srow], w=[r_srow])
        S.op("dve", lambda E: E.tensor_tensor(out=srow[:, 3:4], in0=srow[:, 5:6], in1=srow[:, 4:5], op=ALU.subtract), r=[r_srow], w=[r_srow])
        S.op("dve", lambda E: E.tensor_scalar(out=srow[:, 3:4], in0=srow[:, 3:4], scalar1=-float(lam_init), scalar2=None, op0=ALU.add), r=[r_srow], w=[r_srow])
        S.op("dve", lambda E: E.tensor_scalar(out=srow[:, 0:1], in0=prow[:, P_FFB:P_FFB + 1], scalar1=-1.0, scalar2=None, op0=ALU.mult), r=[r_prow, r_srow], w=[r_srow])
        S.op("dve", lambda E: E.tensor_copy(out=srow[:, 1:2], in_=prow[:, P_DTB:P_DTB + 1]), r=[r_prow, r_srow], w=[r_srow])
        S.op("act", lambda E: E.activation(out=srow[:, 2:3], in_=prow[:, P_ALOG:P_ALOG + 1], func=AF.Exp), r=[r_prow, r_srow], w=[r_srow])
        S.op("dve", lambda E: E.tensor_scalar(out=srow[:, 2:3], in0=srow[:, 2:3], scalar1=-1.0, scalar2=None, op0=ALU.mult), r=[r_srow], w=[r_srow])
        S.op("pe", lambda E: E.matmul(pbc[:], lhsT=onesf[0:1, :], rhs=srow[:], start=True, stop=True), r=[r_srow, r_c], w=[r_pbc])
        S.op("act", lambda E: E.copy(out=pcol[:], in_=pbc[:]), r=[r_pbc], w=[r_pcol])
        if first:
            gb_, bb_ = sb0("lngb", [128, D]), sb0("lnbb", [128, D])
            st, mv, rstd = sb0("st", [128, 2, 6]), sb0("mv", [128, 2]), sb0("rstd", [128, 1])
            xn = [sb0("xn%d" % i, [128, D]) for i in range(2)]
            r_xn, r_g, r_ln = [Res(), Res()], Res(), Res()
            lnt = {"st": st, "mv": mv, "rstd": rstd, "res": r_ln, "eps": c_eps5, "eps_r": r_c}
            S.dma("sp", gb_[:], lng.partition_broadcast(128), w=[r_g])
            S.dma("sp", bb_[:], lnb.partition_broadcast(128), w=[r_g])
        for T in range(NTL):
            k = T % 2
            if first:
                if T == 0:
                    S.op("pool", lambda E: E.memset(xin[0][:], 0.0), w=[r_xin[0]])
                    S.dma("sp", xin[0][112:128, :], meta[:, :], w=[r_xin[0]])
                else:
                    S.dma("sp", xin[k][:], x[(T - 1) * 128:T * 128, :], w=[r_xin[k]])
                _layernorm_tile(S, lnt, xin[k][:], xn[k][:], gb_, bb_, [r_xin[k]], [r_xn[k]], r_g)
                S.dma("sp", s32o[T * 128:(T + 1) * 128, :], xn[k][:], r=[r_xn[k]], is_out=True)
                src, rsrc = xn[k], r_xn[k]
            else:
                S.dma("sp", xin[k][:], sbf[T * 128:(T + 1) * 128, :], w=[r_xin[k]])
                src, rsrc = xin[k], r_xin[k]
            S.op("act", lambda E: E.copy(out=xb[k][:], in_=src[:]), r=[rsrc], w=[r_xb[k]])
            if T == 0:
                S.op("pool", lambda E: E.memset(xb[k][0:112, :], 0.0), r=[r_xb[k]], w=[r_xb[k]])
            for c in range(8):
                S.op("pe", lambda E, c=c: E.transpose(out=tp[:, c, :], in_=xb[k][:, c * 128:(c + 1) * 128], identity=identb[:]), r=[r_xb[k], r_c], w=[r_tp])
            S.op("dve", lambda E: E.tensor_copy(out=xTt[k][:], in_=tp[:]), r=[r_tp], w=[r_xTt[k]])
            S.dma("sp", xTd[:, :, T * 128:(T + 1) * 128].rearrange("c p t -> p c t"), xTt[k][:], r=[r_xTt[k]], w=[r_xTd])
    S.barrier()

    def attention(kT, qT, base, vaug, dvp1, G, maskb, btab, r_in, handler, psS, psO, pT, r_psS, r_psO, r_pT):
        ngroups = (NTL + G - 1) // G
        it = 0
        for g in range(ngroups):
            gb0 = g * G
            Gc = min(G, NTL - gb0)
            Oi, r_Oi = psO[g % 2], r_psO[g % 2]
            firstmm = True
            for kb in range(gb0 + Gc):
                smin = max(0, kb - gb0)
                ncol = (Gc - smin) * 128
                i3 = it % 3
                it += 1
                Sp, Pt = psS[i3], pT[i3]
                S.op("pe", lambda E: E.matmul(Sp[:, 0:ncol], lhsT=kT[base:base + 64, kb * 128:(kb + 1) * 128], rhs=qT[base:base + 64, (gb0 + smin) * 128:(gb0 + Gc) * 128], start=True, stop=True), r=r_in, w=[r_psS[i3]])
                if btab is None:
                    S.op("act", lambda E: E.activation(out=Pt[:, 0:ncol], in_=Sp[:, 0:ncol], func=AF.Exp), r=[r_psS[i3]], w=[r_pT[i3]])
                else:
                    for s in range(smin, Gc):
                        cs = (s - smin) * 128
                        S.op("act", lambda E, s=s, cs=cs: E.activation(out=Pt[:, cs:cs + 128], in_=Sp[:, cs:cs + 128], func=AF.Exp, bias=btab[:, kb, gb0 + s:gb0 + s + 1], scale=1.0), r=[r_psS[i3]] + r_in, w=[r_pT[i3]])
                if kb >= gb0:
                    S.op("pool", lambda E: E.tensor_tensor(out=Pt[:, 0:128], in0=Pt[:, 0:128], in1=maskb[:], op=ALU.mult), r=[r_c], w=[r_pT[i3]])
                for s in range(smin, Gc):
                    cs = (s - smin) * 128
                    S.op("pe", lambda E, s=s, cs=cs: E.matmul(Oi[:, s, 0:dvp1], lhsT=Pt[:, cs:cs + 128], rhs=vaug[:, kb, 0:dvp1], start=firstmm, stop=(kb == gb0 + s), skip_group_check=True), r=[r_pT[i3]] + r_in, w=[r_Oi])
                    firstmm = False
            handler(gb0, Gc, Oi, r_Oi)

    def load_chunk(xTc, r_x, t0, N, halo=0):
        if halo and t0 == 0:
            S.op("pool", lambda E: E.memset(xTc[:, :, 0:halo], 0.0), w=[r_x])
            S.dma("sp", xTc[:, :, halo:halo + N], xTd[:, :, 0:N].rearrange("c p t -> p c t"), r=[r_xTd], w=[r_x])
        else:
            S.dma("sp", xTc[:, :, 0:N + halo], xTd[:, :, t0 - halo:t0 + N].rearrange("c p t -> p c t"), r=[r_xTd], w=[r_x])

    if 1 in stages:
      with contextlib.ExitStack() as e1:
        sb1 = lambda name, shape, dt=F32: e1.enter_context(nc.sbuf_tensor("a1_" + name, shape, dt))
        ps1 = lambda name, shape, dt=F32: e1.enter_context(nc.psum_tensor("a1_" + name, shape, dt))
        dqT, dkT = sb1("dqT", [128, L], BF16), sb1("dkT", [128, L], BF16)
        dva = sb1("dva", [128, NTL, 129], BF16)
        O0 = sb1("O0", [128, NTL, 129])
        xTc = [sb1("xTc%d" % i, [128, 8, 512], BF16) for i in range(2)]
        pT = [sb1("pT%d" % i, [128, 512], BF16) for i in range(3)]
        gsub = sb1("gsub", [128, 128])
        rd = sb1("rd", [128, 8])
        t1 = sb1("t1", [128, 128])
        ot = sb1("ot", [128, 128])
        junk = sb1("junk", [128, 128])
        ss = sb1("ss", [128, 1])
        psS = [ps1("psS%d" % i, [128, 512]) for i in range(3)]
        psO = [ps1("psO%d" % i, [128, 3, 129]) for i in range(2)]
        pq, pk, pv = ps1("pq", [128, 512]), ps1("pk", [128, 512]), ps1("pv", [128, 512])
        r_in, r_xc, r_pT, r_psS, r_psO = Res(), [Res(), Res()], [Res() for _ in range(3)], [Res() for _ in range(3)], [Res(), Res()]
        r_pq, r_pk, r_pv, r_O0, r_gs, r_h = Res(), Res(), Res(), Res(), Res(), Res()
        S.dma("sp", gsub[:], prm[:, P_DSG:P_DSG + 128].partition_broadcast(128), w=[r_gs])
        S.op("act", lambda E: E.mul(gsub[:], gsub[:], float(1.0 - lam_init)), r=[r_gs], w=[r_gs])
        S.op("pool", lambda E: E.memset(dva[:, :, 128:129], 1.0), w=[r_in])
        for ci, (t0, N) in enumerate(chunks):
            xc, r_x = xTc[ci % 2], r_xc[ci % 2]
            load_chunk(xc, r_x, t0, N)
            for c in range(8):
                S.op("pe", lambda E, c=c: E.matmul(pq[:, 0:N], lhsT=wbf[:, c, C_DQ:C_DQ + 128], rhs=xc[:, c, 0:N], start=(c == 0), stop=(c == 7)), r=[r_w, r_x], w=[r_pq])
            S.op("act", lambda E: E.mul(dqT[:, t0:t0 + N], pq[:, 0:N], 0.125), r=[r_pq], w=[r_in])
            for c in range(8):
                S.op("pe", lambda E, c=c: E.matmul(pk[:, 0:N], lhsT=wbf[:, c, C_DK:C_DK + 128], rhs=xc[:, c, 0:N], start=(c == 0), stop=(c == 7)), r=[r_w, r_x], w=[r_pk])
            S.op("dve", lambda E: E.tensor_copy(out=dkT[:, t0:t0 + N], in_=pk[:, 0:N]), r=[r_pk], w=[r_in])
            for s in range(N // 128):
                T = t0 // 128 + s
                for c in range(8):
                    S.op("pe", lambda E, c=c, s=s: E.matmul(pv[:, 0:128], lhsT=xc[:, c, s * 128:(s + 1) * 128], rhs=wbf[:, c, C_DV:C_DV + 128], start=(c == 0), stop=(c == 7)), r=[r_w, r_x], w=[r_pv])
                S.op("act", lambda E, T=T: E.copy(out=dva[:, T, 0:128], in_=pv[:, 0:128]), r=[r_pv], w=[r_in])
        S.op("pool", lambda E: E.memset(dva[0:112, 0, :], 0.0), r=[r_in], w=[r_in])

        def h0(gb0, Gc, Oi, r_Oi):
            S.op("act", lambda E: E.copy(out=O0[:, gb0:gb0 + Gc, :], in_=Oi[:, 0:Gc, :]), r=[r_Oi], w=[r_O0])

        def h1(gb0, Gc, Oi, r_Oi):
            S.op("dve", lambda E: E.reciprocal(out=rd[:, 0:Gc], in_=O0[:, gb0:gb0 + Gc, 128]), r=[r_O0], w=[r_h])
            S.op("dve", lambda E: E.reciprocal(out=rd[:, 4:4 + Gc], in_=Oi[:, 0:Gc, 128]), r=[r_Oi, r_h], w=[r_h])
            S.op("dve", lambda E: E.tensor_scalar(out=rd[:, 4:4 + Gc], in0=rd[:, 4:4 + Gc], scalar1=pcol[:, 3:4], scalar2=None, op0=ALU.mult), r=[r_h, r_pcol], w=[r_h])
            for s in range(Gc):
                T = gb0 + s
                S.op("dve", lambda E, s=s: E.tensor_scalar(out=t1[:], in0=Oi[:, s, 0:128], scalar1=rd[:, 4 + s:5 + s], scalar2=None, op0=ALU.mult), r=[r_Oi, r_h], w=[r_h])
                S.op("dve", lambda E, s=s, T=T: E.scalar_tensor_tensor(out=ot[:], in0=O0[:, T, 0:128], scalar=rd[:, s:s + 1], in1=t1[:], op0=ALU.mult, op1=ALU.add), r=[r_O0, r_h], w=[r_h])
                S.op("act", lambda E: E.activation(out=junk[:], in_=ot[:], func=AF.Square, accum_out=ss[:]), r=[r_h], w=[r_h])
                S.op("act", lambda E: E.activation(out=ss[:], in_=ss[:], func=AF.Sqrt, bias=c_eps5[:], scale=1.0 / 128), r=[r_h, r_c], w=[r_h])
                S.op("dve", lambda E: E.reciprocal(out=ss[:], in_=ss[:]), r=[r_h], w=[r_h])
                S.op("dve", lambda E, T=T: E.scalar_tensor_tensor(out=ybuf[:, T, 0:128], in0=ot[:], scalar=ss[:], in1=gsub[:], op0=ALU.mult, op1=ALU.mult), r=[r_h, r_gs], w=[r_y[T]])

        attention(dkT, dqT, 0, dva, 129, 3, dmaskb, None, [r_in], h0, psS, psO, pT, r_psS, r_psO, r_pT)
        attention(dkT, dqT, 64, dva, 129, 3, dmaskb, None, [r_in], h1, psS, psO, pT, r_psS, r_psO, r_pT)
      S.barrier()

    if 2 in stages:
      with contextlib.ExitStack() as e2:
        sb2 = lambda name, shape, dt=F32: e2.enter_context(nc.sbuf_tensor("a2_" + name, shape, dt))
        ps2 = lambda name, shape, dt=F32: e2.enter_context(nc.psum_tensor("a2_" + name, shape, dt))
        fqT, fkT = sb2("fqT", [64, L], BF16), sb2("fkT", [64, L], BF16)
        fva = sb2("fva", [128, NTL, 65], BF16)
        ffl = sb2("ffl", [128, NTL])
        Cn = sb2("Cn", [128, NTL])
        tots = sb2("tots", [128, NTL])
        excl = sb2("excl", [128, NTL])
        onesr = sb2("onesr", [128, NTL])
        btab = sb2("btab", [128, NTL, NTL])
        xTc = [sb2("xTc%d" % i, [128, 8, 512], BF16) for i in range(2)]
        pT = [sb2("pT%d" % i, [128, 512], BF16) for i in range(3)]
        rd = sb2("rd", [128, 4])
        psS = [ps2("psS%d" % i, [128, 512]) for i in range(3)]
        psO = [ps2("psO%d" % i, [128, 4, 128]) for i in range(2)]
        pq, pk, pv = ps2("pq", [128, 512]), ps2("pk", [128, 512]), ps2("pv", [128, 512])
        r_in, r_xc, r_pT, r_psS, r_psO = Res(), [Res(), Res()], [Res() for _ in range(3)], [Res() for _ in range(3)], [Res(), Res()]
        r_pq, r_pk, r_pv, r_f, r_h = Res(), Res(), Res(), Res(), Res()
        S.op("pool", lambda E: E.memset(fva[:, :, 64:65], 1.0), w=[r_in])
        S.op("pool", lambda E: E.memset(onesr[:], 1.0), w=[r_f])
        for ci, (t0, N) in enumerate(chunks):
            xc, r_x = xTc[ci % 2], r_xc[ci % 2]
            load_chunk(xc, r_x, t0, N)
            for c in range(8):
                S.op("pe", lambda E, c=c: E.matmul(pq[:, 0:N], lhsT=wbf[:, c, C_FQ:C_FQ + 128], rhs=xc[:, c, 0:N], start=(c == 0), stop=(c == 7)), r=[r_w, r_x], w=[r_pq])
            S.op("act", lambda E: E.mul(fqT[:, t0:t0 + N], pq[0:64, 0:N], 0.125), r=[r_pq], w=[r_in])
            for c in range(8):
                S.op("pe", lambda E, c=c: E.matmul(pk[:, 0:N], lhsT=wbf[:, c, C_FK:C_FK + 128], rhs=xc[:, c, 0:N], start=(c == 0), stop=(c == 7)), r=[r_w, r_x], w=[r_pk])
            S.op("dve", lambda E: E.tensor_copy(out=fkT[:, t0:t0 + N], in_=pk[0:64, 0:N]), r=[r_pk], w=[r_in])
            for s in range(N // 128):
                T = t0 // 128 + s
                for c in range(8):
                    S.op("pe", lambda E, c=c, s=s: E.matmul(pv[:, 0:65], lhsT=xc[:, c, s * 128:(s + 1) * 128], rhs=wbf[:, c, C_FV:C_FV + 65], start=(c == 0), stop=(c == 7)), r=[r_w, r_x], w=[r_pv])
                S.op("act", lambda E, T=T: E.copy(out=fva[:, T, 0:64], in_=pv[:, 0:64]), r=[r_pv], w=[r_in])
                S.op("dve", lambda E, T=T: E.tensor_copy(out=ffl[:, T:T + 1], in_=pv[:, 64:65]), r=[r_pv], w=[r_f])
        S.op("pool", lambda E: E.memset(fva[0:112, 0, :], 0.0), r=[r_in], w=[r_in])
        if os.environ.get("FOXDBG") == "4":
            raise_skip = True
        else:
            raise_skip = False
        if not raise_skip:
            S.op("act", lambda E: E.activation(out=ffl[:], in_=ffl[:], func=AF.Exp, bias=pcol[:, 0:1], scale=-1.0), r=[r_f, r_pcol], w=[r_f])
            S.op("act", lambda E: E.activation(out=ffl[:], in_=ffl[:], func=AF.Ln, bias=c_one[:], scale=1.0), r=[r_f, r_c], w=[r_f])
            S.op("pe", lambda E: E.matmul(pq[:, 0:NTL], lhsT=trif[:], rhs=ffl[:], start=True, stop=True), r=[r_f, r_c], w=[r_pq])
            S.op("pe", lambda E: E.matmul(pk[:, 0:NTL], lhsT=onesf[:], rhs=ffl[:], start=True, stop=True), r=[r_f, r_c], w=[r_pk])
            S.op("dve", lambda E: E.tensor_copy(out=tots[:], in_=pk[:, 0:NTL]), r=[r_pk], w=[r_f])

            if os.environ.get("FOXDBG") == "1":
                S.op("dve", lambda E: E.tensor_copy(out=excl[:], in_=tots[:]), r=[r_f], w=[r_f])
            else:
                S.op("dve", lambda E: E.tensor_tensor_scan(out=excl[:], data0=onesr[:], data1=tots[:], initial=0.0, op0=ALU.mult, op1=ALU.add), r=[r_f], w=[r_f])
            S.op("dve", lambda E: E.tensor_tensor(out=excl[:], in0=excl[:], in1=tots[:], op=ALU.subtract), r=[r_f], w=[r_f])
            S.op("dve", lambda E: E.tensor_tensor(out=Cn[:], in0=pq[:, 0:NTL], in1=excl[:], op=ALU.add), r=[r_pq, r_f], w=[r_f])
            for kb in range(NTL):
                S.op("dve", lambda E, kb=kb: E.tensor_scalar(out=btab[:, kb, :], in0=excl[:], scalar1=Cn[:, kb:kb + 1], scalar2=-1.0, op0=ALU.subtract, op1=ALU.mult), r=[r_f], w=[r_in])

        def hf(gb0, Gc, Oi, r_Oi):
            S.op("dve", lambda E: E.reciprocal(out=rd[:, 0:Gc], in_=Oi[:, 0:Gc, 64]), r=[r_Oi], w=[r_h])
            for s in range(Gc):
                T = gb0 + s
                S.op("dve", lambda E, s=s, T=T: E.tensor_scalar(out=ybuf[:, T, 128:192], in0=Oi[:, s, 0:64], scalar1=rd[:, s:s + 1], scalar2=None, op0=ALU.mult), r=[r_Oi, r_h], w=[r_y[T]])

        if os.environ.get("FOXDBG") != "3":
          attention(fkT, fqT, 0, fva, 65, 4, cmaskb, None if os.environ.get("FOXDBG") == "2" else btab, [r_in], hf, psS, psO, pT, r_psS, r_psO, r_pT)
      S.barrier()

    if 3 in stages:
      with contextlib.ExitStack() as e3:
        sb3 = lambda name, shape, dt=F32: e3.enter_context(nc.sbuf_tensor("a3_" + name, shape, dt))
        ps3 = lambda name, shape, dt=F32: e3.enter_context(nc.psum_tensor("a3_" + name, shape, dt))
        Wc = [sb3("Wc%d" % j, [128, 8, 192], BF16) for j in range(4)]
        wcr = sb3("wcr", [128, 4, 192])
        sq = sb3("sq", [128, NTL, 192])
        gzz = sb3("gzz", [128, NTL, 66])
        ogd = sb3("ogd", [128, NTL, 64])
        xTc = [sb3("xTc%d" % i, [128, 8, 515], BF16) for i in range(2)]
        ssq = sb3("ssq", [128, NTL, 2])
        nbeta, bke, gg, gc, gtot, egc, ekd, ecd = [sb3(n, [128, NTL]) for n in ("nbeta", "bke", "gg", "gc", "gtot", "egc", "ekd", "ecd")]
        beta = sb3("beta", [128, NTL])
        gngb = sb3("gngb", [128, 64])
        kn, qn, kd = sb3("kn", [128, 64]), sb3("qn", [128, 64]), sb3("kd", [128, 64])
        knT, qnT, qdT, egr, wT = [sb3(n, [64, 128]) for n in ("knT", "qnT", "qdT", "egr", "wT")]
        gtri, Dm, DmT, Bm, BT, Bn, BTn, Rm, TTb, TTw, qkT = [sb3(n, [128, 128]) for n in ("gtri", "Dm", "DmT", "Bm", "BT", "Bn", "BTn", "Rm", "TTb", "TTw", "qkT")]
        u, vnew = sb3("u", [128, 64]), sb3("vnew", [128, 64])
        St = sb3("St", [64, 64])
        cmaskf = trif
        pc, pz, po = ps3("pc", [128, 512]), ps3("pz", [128, 512]), ps3("po", [128, 512])
        px = [ps3("px%d" % i, [128, 512]) for i in range(5)]
        r_px = [Res() for _ in range(5)]
        r_pc, r_pz, r_po = Res(), Res(), Res()
        r_Wc, r_sq, r_gzz, r_xc, r_b, r_S, r_og = Res(), [Res() for _ in range(NTL)], Res(), [Res(), Res()], Res(), Res(), [Res() for _ in range(NTL)]
        r_t = {n: Res() for n in ("kn", "qn", "kd", "knT", "qnT", "qdT", "egr", "wT", "gtri", "Dm", "DmT", "Bm", "BT", "Bn", "BTn", "Rm", "TTb", "TTw", "qkT", "u", "vnew")}
        ipx = [0]

        def nx():
            i = ipx[0] % 5
            ipx[0] += 1
            return px[i], r_px[i]

        S.dma("sp", wcr[:].rearrange("p a b -> p (a b)"), prm[:, P_GCW:P_GCW + 768].partition_broadcast(128), w=[r_Wc])
        S.dma("sp", gngb[:], prm[:, P_GNG:P_GNG + 64].partition_broadcast(128), w=[r_b])
        for j in range(4):
            for c in range(8):
                S.op("dve", lambda E, j=j, c=c: E.tensor_tensor(out=Wc[j][:, c, :], in0=wbf[:, c, C_GQ:C_GQ + 192], in1=wcr[:, j, :], op=ALU.mult), r=[r_w, r_Wc], w=[r_Wc])
        for ci, (t0, N) in enumerate(chunks):
            xc, r_x = xTc[ci % 2], r_xc[ci % 2]
            load_chunk(xc, r_x, t0, N, halo=3)
            for s in range(N // 128):
                T = t0 // 128 + s
                n = 0
                for j in range(4):
                    for c in range(8):
                        S.op("pe", lambda E, j=j, c=c, s=s, n=n: E.matmul(pc[:, 0:192], lhsT=xc[:, c, s * 128 + j:s * 128 + j + 128], rhs=Wc[j][:, c, :], start=(n == 0), stop=(n == 31)), r=[r_Wc, r_x], w=[r_pc])
                        n += 1
                S.op("act", lambda E, T=T: E.activation(out=sq[:, T, :], in_=pc[:, 0:192], func=AF.Silu), r=[r_pc], w=[r_sq[T]])
                for c in range(8):
                    S.op("pe", lambda E, c=c, s=s: E.matmul(pz[:, 0:66], lhsT=xc[:, c, 3 + s * 128:3 + (s + 1) * 128], rhs=wbf[:, c, C_GZ:C_GZ + 66], start=(c == 0), stop=(c == 7)), r=[r_w, r_x], w=[r_pz])
                S.op("dve", lambda E, T=T: E.tensor_copy(out=gzz[:, T, :], in_=pz[:, 0:66]), r=[r_pz], w=[r_gzz])
        for qk in range(2):
            S.op("dve", lambda E, qk=qk: E.tensor_tensor(out=ogd[:], in0=sq[:, :, qk * 64:(qk + 1) * 64], in1=sq[:, :, qk * 64:(qk + 1) * 64], op=ALU.mult), r=r_sq, w=[r_b])
            S.op("dve", lambda E, qk=qk: E.tensor_reduce(out=ssq[:, :, qk], in_=ogd[:], axis=AX.X, op=ALU.add), r=[r_b], w=[r_b])
        S.op("act", lambda E: E.activation(out=ssq[:], in_=ssq[:], func=AF.Sqrt, bias=c_eps6[:], scale=1.0), r=[r_b, r_c], w=[r_b])
        S.op("dve", lambda E: E.reciprocal(out=ssq[:], in_=ssq[:]), r=[r_b], w=[r_b])
        S.op("act", lambda E: E.activation(out=beta[:], in_=gzz[:, :, 64], func=AF.Sigmoid), r=[r_gzz], w=[r_b])
        S.op("dve", lambda E: E.tensor_scalar(out=nbeta[:], in0=beta[:], scalar1=-1.0, scalar2=None, op0=ALU.mult), r=[r_b], w=[r_b])
        S.op("act", lambda E: E.activation(out=gg[:], in_=gzz[:, :, 65], func=AF.Exp, bias=pcol[:, 1:2], scale=1.0), r=[r_gzz, r_pcol], w=[r_b])
        S.op("act", lambda E: E.activation(out=gg[:], in_=gg[:], func=AF.Ln, bias=c_one[:], scale=1.0), r=[r_b, r_c], w=[r_b])
        S.op("dve", lambda E: E.tensor_scalar(out=gg[:], in0=gg[:], scalar1=pcol[:, 2:3], scalar2=None, op0=ALU.mult), r=[r_b, r_pcol], w=[r_b])
        pa, r_pa = nx()
        S.op("pe", lambda E: E.matmul(pa[:, 0:NTL], lhsT=trif[:], rhs=gg[:], start=True, stop=True), r=[r_b, r_c], w=[r_pa])
        S.op("dve", lambda E: E.tensor_copy(out=gc[:], in_=pa[:, 0:NTL]), r=[r_pa], w=[r_b])
        pa2, r_pa2 = nx()
        S.op("pe", lambda E: E.matmul(pa2[:, 0:NTL], lhsT=onesf[:], rhs=gg[:], start=True, stop=True), r=[r_b, r_c], w=[r_pa2])
        S.op("dve", lambda E: E.tensor_copy(out=gtot[:], in_=pa2[:, 0:NTL]), r=[r_pa2], w=[r_b])
        S.op("act", lambda E: E.activation(out=egc[:], in_=gc[:], func=AF.Exp), r=[r_b], w=[r_b])
        S.op("act", lambda E: E.activation(out=ecd[:], in_=gtot[:], func=AF.Exp), r=[r_b], w=[r_b])
        S.op("dve", lambda E: E.tensor_tensor(out=ekd[:], in0=gtot[:], in1=gc[:], op=ALU.subtract), r=[r_b], w=[r_b])
        S.op("act", lambda E: E.activation(out=ekd[:], in_=ekd[:], func=AF.Exp), r=[r_b], w=[r_b])
        S.op("dve", lambda E: E.tensor_tensor(out=bke[:], in0=beta[:], in1=egc[:], op=ALU.mult), r=[r_b], w=[r_b])
        S.op("dve", lambda E: E.memset(St[:], 0.0), w=[r_S])

        def tr(dst, rdst, src, rsrc, npart, nfree):
            p, r_p = nx()
            S.op("pe", lambda E: E.transpose(out=p[0:nfree, 0:npart], in_=src, identity=identf[0:npart, 0:npart]), r=rsrc + [r_c], w=[r_p])
            S.op("act", lambda E: E.copy(out=dst, in_=p[0:nfree, 0:npart]), r=[r_p], w=[rdst])

        for T in range(NTL):
            rt = r_t
            S.op("dve", lambda E: E.tensor_scalar(out=kn[:], in0=sq[:, T, 64:128], scalar1=ssq[:, T, 1:2], scalar2=None, op0=ALU.mult), r=[r_sq[T], r_b], w=[rt["kn"]])
            S.op("dve", lambda E: E.tensor_scalar(out=qn[:], in0=sq[:, T, 0:64], scalar1=ssq[:, T, 0:1], scalar2=0.125, op0=ALU.mult, op1=ALU.mult), r=[r_sq[T], r_b], w=[rt["qn"]])
            tr(knT[:], rt["knT"], kn[:], [rt["kn"]], 128, 64)
            tr(qnT[:], rt["qnT"], qn[:], [rt["qn"]], 128, 64)
            S.op("dve", lambda E: E.tensor_scalar(out=gtri[:], in0=trif[:], scalar1=gg[:, T:T + 1], scalar2=None, op0=ALU.mult), r=[r_b, r_c], w=[rt["gtri"]])
            pg, r_pg = nx()
            S.op("pe", lambda E: E.matmul(pg[:, 0:128], lhsT=onesf[:], rhs=gtri[:], start=True, stop=True), r=[rt["gtri"], r_c], w=[r_pg])
            S.op("dve", lambda E: E.tensor_scalar(out=Dm[:], in0=pg[:, 0:128], scalar1=gc[:, T:T + 1], scalar2=0.0, op0=ALU.subtract, op1=ALU.max), r=[r_pg, r_b], w=[rt["Dm"]])
            S.op("act", lambda E: E.activation(out=Dm[:], in_=Dm[:], func=AF.Exp, scale=-1.0), r=[rt["Dm"]], w=[rt["Dm"]])
            S.op("dve", lambda E: E.tensor_scalar(out=DmT[:], in0=pg[:, 0:128], scalar1=gc[:, T:T + 1], scalar2=0.0, op0=ALU.subtract, op1=ALU.min), r=[r_pg, r_b], w=[rt["DmT"]])
            S.op("act", lambda E: E.activation(out=DmT[:], in_=DmT[:], func=AF.Exp), r=[rt["DmT"]], w=[rt["DmT"]])
            S.op("pool", lambda E: E.tensor_tensor(out=DmT[:], in0=DmT[:], in1=cmaskf[:], op=ALU.mult), r=[rt["DmT"], r_c], w=[rt["DmT"]])
            S.op("act", lambda E: E.activation(out=egr[:], in_=pg[0:64, 0:128], func=AF.Exp), r=[r_pg], w=[rt["egr"]])
            pkk, r_pkk = nx()
            S.op("pe", lambda E: E.matmul(pkk[:, 0:128], lhsT=knT[:], rhs=knT[:], start=True, stop=True), r=[rt["knT"]], w=[r_pkk])
            S.op("dve", lambda E: E.tensor_tensor(out=Bm[:], in0=pkk[:, 0:128], in1=Dm[:], op=ALU.mult), r=[r_pkk, rt["Dm"]], w=[rt["Bm"]])
            S.op("dve", lambda E: E.scalar_tensor_tensor(out=Bm[:], in0=Bm[:], scalar=nbeta[:, T:T + 1], in1=smask[:], op0=ALU.mult, op1=ALU.mult), r=[rt["Bm"], r_b, r_c], w=[rt["Bm"]])
            tr(BT[:], rt["BT"], Bm[:], [rt["Bm"]], 128, 128)
            S.op("pool", lambda E: E.tensor_tensor(out=Rm[:], in0=BT[:], in1=identf[:], op=ALU.add), r=[rt["BT"], r_c], w=[rt["Rm"]])
            cb, cbt, rcb, rcbt = Bm, BT, rt["Bm"], rt["BT"]
            nb_, nbt, rnb, rnbt = Bn, BTn, rt["Bn"], rt["BTn"]
            for lev in range(6):
                p1, r_p1 = nx()
                p2, r_p2 = nx()
                S.op("pe", lambda E: E.matmul(p1[:, 0:128], lhsT=cbt[:], rhs=cb[:], start=True, stop=True), r=[rcb, rcbt], w=[r_p1])
                S.op("pe", lambda E: E.matmul(p2[:, 0:128], lhsT=cb[:], rhs=cbt[:], start=True, stop=True), r=[rcb, rcbt], w=[r_p2])
                S.op("act", lambda E: E.copy(out=nb_[:], in_=p1[:, 0:128]), r=[r_p1], w=[rnb])
                S.op("dve", lambda E: E.tensor_copy(out=nbt[:], in_=p2[:, 0:128]), r=[r_p2], w=[rnbt])
                p3, r_p3 = nx()
                S.op("pe", lambda E: E.matmul(p3[:, 0:128], lhsT=nb_[:], rhs=Rm[:], start=True, stop=True), r=[rnb, rt["Rm"]], w=[r_p3])
                S.op("dve", lambda E: E.tensor_tensor(out=Rm[:], in0=Rm[:], in1=p3[:, 0:128], op=ALU.add), r=[r_p3], w=[rt["Rm"]])
                cb, cbt, rcb, rcbt, nb_, nbt, rnb, rnbt = nb_, nbt, rnb, rnbt, cb, cbt, rcb, rcbt
            S.op("dve", lambda E: E.tensor_scalar(out=TTb[:], in0=Rm[:], scalar1=beta[:, T:T + 1], scalar2=None, op0=ALU.mult), r=[rt["Rm"], r_b], w=[rt["TTb"]])
            S.op("pool", lambda E: E.tensor_scalar(out=TTw[:], in0=Rm[:], scalar1=bke[:, T:T + 1], scalar2=None, op0=ALU.mult), r=[rt["Rm"], r_b], w=[rt["TTw"]])
            pu, r_pu = nx()
            S.op("pe", lambda E: E.matmul(pu[:, 0:64], lhsT=TTb[:], rhs=sq[:, T, 128:192], start=True, stop=True), r=[rt["TTb"], r_sq[T]], w=[r_pu])
            S.op("act", lambda E: E.copy(out=u[:], in_=pu[:, 0:64]), r=[r_pu], w=[rt["u"]])
            pw, r_pw = nx()
            S.op("pe", lambda E: E.matmul(pw[0:64, 0:128], lhsT=kn[:], rhs=TTw[:], start=True, stop=True), r=[rt["kn"], rt["TTw"]], w=[r_pw])
            S.op("act", lambda E: E.copy(out=wT[:], in_=pw[0:64, 0:128]), r=[r_pw], w=[rt["wT"]])
            pqk, r_pqk = nx()
            S.op("pe", lambda E: E.matmul(pqk[:, 0:128], lhsT=knT[:], rhs=qnT[:], start=True, stop=True), r=[rt["knT"], rt["qnT"]], w=[r_pqk])
            S.op("dve", lambda E: E.tensor_tensor(out=qkT[:], in0=pqk[:, 0:128], in1=DmT[:], op=ALU.mult), r=[r_pqk, rt["DmT"]], w=[rt["qkT"]])
            S.op("dve", lambda E: E.tensor_tensor(out=qdT[:], in0=qnT[:], in1=egr[:], op=ALU.mult), r=[rt["qnT"], rt["egr"]], w=[rt["qdT"]])
            S.op("pool", lambda E: E.tensor_scalar(out=kd[:], in0=kn[:], scalar1=ekd[:, T:T + 1], scalar2=None, op0=ALU.mult), r=[rt["kn"], r_b], w=[rt["kd"]])
            pvn, r_pvn = nx()
            S.op("pe", lambda E: E.matmul(pvn[:, 0:64], lhsT=wT[:], rhs=St[:], start=True, stop=True), r=[rt["wT"], r_S], w=[r_pvn])
            S.op("dve", lambda E: E.tensor_tensor(out=vnew[:], in0=u[:], in1=pvn[:, 0:64], op=ALU.subtract), r=[rt["u"], r_pvn], w=[rt["vnew"]])
            S.op("pe", lambda E: E.matmul(po[:, 0:64], lhsT=qdT[:], rhs=St[:], start=True, stop=False), r=[rt["qdT"], r_S], w=[r_po])
            S.op("pe", lambda E: E.matmul(po[:, 0:64], lhsT=qkT[:], rhs=vnew[:], start=False, stop=True), r=[rt["qkT"], rt["vnew"]], w=[r_po])
            S.op("act", lambda E: E.copy(out=ogd[:, T, :], in_=po[:, 0:64]), r=[r_po, r_b], w=[r_og[T]])
            pS, r_pS = nx()
            S.op("pe", lambda E: E.matmul(pS[0:64, 0:64], lhsT=kd[:], rhs=vnew[:], start=True, stop=True), r=[rt["kd"], rt["vnew"]], w=[r_pS])
            S.op("dve", lambda E: E.scalar_tensor_tensor(out=St[:], in0=St[:], scalar=ecd[0:64, T:T + 1], in1=pS[0:64, 0:64], op0=ALU.mult, op1=ALU.add), r=[r_pS, r_b], w=[r_S])
        zs = sq
        S.op("act", lambda E: E.activation(out=zs[:, :, 0:64], in_=gzz[:, :, 0:64], func=AF.Silu), r=[r_gzz] + r_og, w=r_sq)
        S.op("dve", lambda E: E.tensor_tensor(out=zs[:, :, 64:128], in0=ogd[:], in1=ogd[:], op=ALU.mult), r=r_og, w=r_sq)
        S.op("dve", lambda E: E.tensor_reduce(out=ssq[:, :, 0], in_=zs[:, :, 64:128], axis=AX.X, op=ALU.add), r=r_sq, w=[r_b])
        S.op("act", lambda E: E.activation(out=ssq[:, :, 0], in_=ssq[:, :, 0], func=AF.Sqrt, bias=c_eps6[:], scale=1.0 / 64), r=[r_b, r_c], w=[r_b])
        S.op("dve", lambda E: E.reciprocal(out=ssq[:, :, 0], in_=ssq[:, :, 0]), r=[r_b], w=[r_b])
        for T in range(NTL):
            S.op("dve", lambda E, T=T: E.scalar_tensor_tensor(out=kn[:], in0=ogd[:, T, :], scalar=ssq[:, T, 0:1], in1=gngb[:], op0=ALU.mult, op1=ALU.mult), r=[r_og[T], r_b], w=[r_t["kn"]])
            S.op("dve", lambda E, T=T: E.tensor_tensor(out=ybuf[:, T, 192:256], in0=kn[:], in1=zs[:, T, 0:64], op=ALU.mult), r=[r_t["kn"]] + r_sq[T:T + 1], w=[r_y[T]])
      S.barrier()

    S.op("pool", lambda E: E.memset(ybuf[0:112, 0, :], 0.0), r=[r_y[0]], w=[r_y[0]])
    for T in range(NTL):
        S.dma("sp", y[T * 128:(T + 1) * 128, :], ybuf[:, T, :], r=[r_y[T]], is_out=True)
    S.finish()
    return nc


def _lam_init(layer):
    import math
    return 0.8 - 0.6 * math.exp(-0.3 * layer)


def _prep_A_weights(inp, layer, j):
    w_in = inp["w_in"][layer]
    wc = np.zeros((D, WC), np.float32)
    wc[:, C_DQ:C_DQ + 128] = w_in[:, j * 128:(j + 1) * 128]
    wc[:, C_DK:C_DK + 128] = w_in[:, 512 + j * 128:512 + (j + 1) * 128]
    wc[:, C_DV:C_DV + 128] = w_in[:, 1024 + j * 128:1024 + (j + 1) * 128]
    wc[:, C_FQ:C_FQ + 64] = w_in[:, 1536 + j * 64:1536 + (j + 1) * 64]
    wc[:, C_FK:C_FK + 64] = w_in[:, 1792 + j * 64:1792 + (j + 1) * 64]
    wc[:, C_FK + 64:C_FK + 128] = w_in[:, 1536 + j * 64:1536 + (j + 1) * 64]
    wc[:, C_FV:C_FV + 64] = w_in[:, 2048 + j * 64:2048 + (j + 1) * 64]
    wc[:, C_FV + 64] = w_in[:, 2304 + j]
    wc[:, C_GQ:C_GQ + 64] = w_in[:, 2308 + j * 64:2308 + (j + 1) * 64]
    wc[:, C_GQ + 64:C_GQ + 128] = w_in[:, 2564 + j * 64:2564 + (j + 1) * 64]
    wc[:, C_GQ + 128:C_GQ + 192] = w_in[:, 2820 + j * 64:2820 + (j + 1) * 64]
    wc[:, C_GZ:C_GZ + 64] = w_in[:, 3084 + j * 64:3084 + (j + 1) * 64]
    wc[:, C_GZ + 64] = w_in[:, 3076 + j]
    wc[:, C_GZ + 65] = w_in[:, 3080 + j]
    prm = np.zeros((1, PRM), np.float32)
    prm[0, P_LAM:P_LAM + 256] = inp["diff_lambda"][layer].reshape(-1)
    prm[0, P_DSG:P_DSG + 128] = inp["diff_subln_g"][layer]
    prm[0, P_FFB] = inp["fox_forget_b"][layer][j]
    prm[0, P_ALOG] = inp["gdn_a_log"][layer][j]
    prm[0, P_DTB] = inp["gdn_dt_bias"][layer][j]
    prm[0, P_GNG:P_GNG + 64] = inp["gdn_norm_g"][layer]
    cw = inp["gdn_conv_w"][layer]
    g = np.concatenate([cw[:, j * 64:(j + 1) * 64], cw[:, 256 + j * 64:256 + (j + 1) * 64], cw[:, 512 + j * 64:512 + (j + 1) * 64]], axis=1)
    prm[0, P_GCW:P_GCW + 768] = g.reshape(-1)
    return wc, prm


def _tiles(arr, r):
    a = arr[TILE_START[r] * 128:TILE_START[r + 1] * 128]
    pad = NTB * 128 - a.shape[0]
    if pad:
        a = np.concatenate([a, a[:pad]], 0)
    return np.ascontiguousarray(a)


def _untile(parts):
    return np.concatenate([parts[r][:(TILE_START[r + 1] - TILE_START[r]) * 128] for r in range(4)], 0)


def kernel(**inp):
    inp = {k: np.asarray(v) for k, v in inp.items()}
    cores = list(range(8))
    nc_B = build_B()
    stream32 = None
    streambf = None
    for layer in range(2):
        nc_A = build_A(layer == 0, _lam_init(layer))
        in_maps = []
        for c in cores:
            b, j = divmod(c, 4)
            wc, prm = _prep_A_weights(inp, layer, j)
            m = {"w": wc, "prm": prm}
            if layer == 0:
                m.update({"x": np.ascontiguousarray(inp["x"][b]), "meta": inp["meta_tokens"],
                          "lng": inp["ln_in_g"][None], "lnb": inp["ln_in_b"][None]})
            else:
                m["sbf"] = streambf[b]
            in_maps.append(m)
        resA = run_bass_kernel_spmd(nc_A, in_maps, core_ids=cores).results
        if layer == 0:
            stream32 = [resA[0]["s32o"], resA[4]["s32o"]]
        yfull = []
        for b in range(2):
            yb = np.zeros((L, D), ml_dtypes.bfloat16)
            for j in range(4):
                yc = resA[4 * b + j]["y"]
                yb[:, j * 128:(j + 1) * 128] = yc[:, 0:128]
                yb[:, 512 + j * 64:512 + (j + 1) * 64] = yc[:, 128:192]
                yb[:, 768 + j * 64:768 + (j + 1) * 64] = yc[:, 192:256]
            yfull.append(yb)
        in_maps = []
        for c in cores:
            b, r = divmod(c, 4)
            in_maps.append({"yg": _tiles(yfull[b], r), "s32": _tiles(stream32[b], r),
                            "wout": inp["w_out"][layer], "ln1g": inp["ln1_g"][layer][None], "ln1b": inp["ln1_b"][layer][None],
                            "ln2g": inp["ln2_g"][layer][None], "ln2b": inp["ln2_b"][layer][None],
                            "rw": inp["router_w"][layer], "rb": inp["router_b"][layer][None],
                            "w1": inp["expert_w1"][layer], "b1": inp["expert_b1"][layer],
                            "w2": inp["expert_w2"][layer], "b2": inp["expert_b2"][layer]})
        resB = run_bass_kernel_spmd(nc_B, in_maps, core_ids=cores).results
        stream32 = [_untile([resB[4 * b + r]["out32"] for r in range(4)]) for b in range(2)]
        streambf = [_untile([resB[4 * b + r]["outbf"] for r in range(4)]) for b in range(2)]
    out = np.stack([stream32[b][128:] for b in range(2)], 0).astype(np.float32)
    return out
```

```python
import contextlib
import os
import numpy as np
import ml_dtypes
import concourse.bass as bass
import concourse.mybir as mybir
from concourse.bass_utils import run_bass_kernel_spmd

F32 = mybir.dt.float32
BF16 = mybir.dt.bfloat16
AF = mybir.ActivationFunctionType
ALU = mybir.AluOpType
AX = mybir.AxisListType

D = 1024
L = 8320
NTL = 65
NTB = 17
ALPHA = 4 ** 0.25
TILE_START = [0, 17, 33, 49, 65]


class Res:
    __slots__ = ("w", "rs")

    def __init__(self):
        self.w = None
        self.rs = {}


class Sched:
    def __init__(self, nc, es, nds=14):
        self.nc = nc
        self.E = {"pe": nc.tensor, "act": nc.scalar, "dve": nc.vector, "pool": nc.gpsimd, "sp": nc.sync}
        self.sem = {k: es.enter_context(nc.semaphore("c_" + k)) for k in ("pe", "act", "dve", "pool")}
        self.cnt = {k: 0 for k in self.sem}
        self.seen = {k: {} for k in self.E}
        self.dsem = [es.enter_context(nc.semaphore("d%d" % i)) for i in range(nds)]
        self.dcnt = [0] * nds
        self.dnext = 0
        self.out_events = []

    def _sem(self, key):
        return self.dsem[key[1]] if isinstance(key, tuple) else self.sem[key]

    def _wait(self, e, ev):
        key, val = ev
        if e == "pe" and key == "pe":
            return
        if self.seen[e].get(key, 0) >= val:
            return
        self.E[e].wait_ge(self._sem(key), val)
        self.seen[e][key] = val

    def _deps(self, e, r, w):
        evs = {}
        for x in r:
            if x.w is not None:
                evs[x.w[0]] = max(evs.get(x.w[0], 0), x.w[1])
        for x in w:
            if x.w is not None:
                evs[x.w[0]] = max(evs.get(x.w[0], 0), x.w[1])
            for k, v in x.rs.items():
                evs[k] = max(evs.get(k, 0), v)
        for k, v in evs.items():
            self._wait(e, (k, v))

    def _post(self, ev, r, w):
        for x in r:
            x.rs[ev[0]] = max(x.rs.get(ev[0], 0), ev[1])
        for x in w:
            x.w = ev
            x.rs = {}

    def op(self, e, fn, r=(), w=()):
        self._deps(e, r, w)
        ins = fn(self.E[e])
        self.cnt[e] += 1
        ins.then_inc(self.sem[e], 1)
        ev = (e, self.cnt[e])
        self._post(ev, r, w)
        return ev

    def dma(self, e, out, in_, r=(), w=(), is_out=False, **kw):
        self._deps(e, r, w)
        i = self.dnext
        self.dnext = (self.dnext + 1) % len(self.dsem)
        key = ("d", i)
        if self.dcnt[i] > 0:
            self._wait(e, (key, 16 * self.dcnt[i]))
        ins = self.E[e].dma_start(out=out, in_=in_, **kw)
        self.dcnt[i] += 1
        ins.then_inc(self.dsem[i], 16)
        ev = (key, 16 * self.dcnt[i])
        self._post(ev, r, w)
        if is_out:
            self.out_events.append(ev)
        return ev

    def barrier(self):
        for e in self.E:
            for k in self.sem:
                if self.cnt[k] > 0:
                    self._wait(e, (k, self.cnt[k])) if not (e == k) else self._wait_self(e)
            for i in range(len(self.dsem)):
                if self.dcnt[i] > 0:
                    self._wait(e, (("d", i), 16 * self.dcnt[i]))

    def _wait_self(self, e):
        if self.seen[e].get(e, 0) < self.cnt[e]:
            self.E[e].wait_ge(self.sem[e], self.cnt[e])
            self.seen[e][e] = self.cnt[e]

    def finish(self):
        evs = {}
        for k, v in self.out_events:
            evs[k] = max(evs.get(k, 0), v)
        for k, v in evs.items():
            self._wait("sp", (k, v))


def _layernorm_tile(S, es_tiles, src, dst, g_bc, b_bc, r_src, w_dst, rg):
    st, mv, rstd = es_tiles["st"], es_tiles["mv"], es_tiles["rstd"]
    rs = es_tiles["res"]
    for h in range(2):
        S.op("dve", lambda E, h=h: E.bn_stats(out=st[:, h, :], in_=src[:, h * 512:(h + 1) * 512]), r=r_src, w=[rs])
    S.op("dve", lambda E: E.bn_aggr(out=mv[:], in_=st[:].rearrange("p a b -> p (a b)")), r=[rs], w=[rs])
    S.op("act", lambda E: E.activation(out=rstd[:], in_=mv[:, 1:2], func=AF.Sqrt, bias=es_tiles["eps"][:], scale=1.0), r=[rs, es_tiles["eps_r"]], w=[rs])
    S.op("dve", lambda E: E.reciprocal(out=rstd[:], in_=rstd[:]), r=[rs], w=[rs])
    S.op("dve", lambda E: E.tensor_scalar(out=dst, in0=src, scalar1=mv[:, 0:1], scalar2=rstd[:], op0=ALU.subtract, op1=ALU.mult), r=list(r_src) + [rs], w=w_dst)
    S.op("dve", lambda E: E.tensor_tensor(out=dst, in0=dst, in1=g_bc[:], op=ALU.mult), r=[rg], w=w_dst)
    S.op("dve", lambda E: E.tensor_tensor(out=dst, in0=dst, in1=b_bc[:], op=ALU.add), r=[rg], w=w_dst)


def build_B(ne=32, debug=False):
    nc = bass.Bass("TRN2", target_bir_lowering=False)
    es = contextlib.ExitStack()
    NTOK = NTB * 128

    def din(name, shape, dt=F32):
        return nc.dram_tensor(name, shape, dt, kind="ExternalInput").ap()

    yg = din("yg", [NTOK, D], BF16)
    s32 = din("s32", [NTOK, D])
    wout = din("wout", [D, D])
    ln1g, ln1b, ln2g, ln2b = din("ln1g", [1, D]), din("ln1b", [1, D]), din("ln2g", [1, D]), din("ln2b", [1, D])
    rw = din("rw", [D, 32])
    rbias = din("rb", [1, 32])
    w1 = din("w1", [max(ne, 1), D, 2 * D])
    b1 = din("b1", [32, 2 * D])
    w2 = din("w2", [max(ne, 1), D, D])
    b2 = din("b2", [32, D])
    out32 = nc.dram_tensor("out32", [NTOK, D], F32, kind="ExternalOutput").ap()
    outbf = nc.dram_tensor("outbf", [NTOK, D], BF16, kind="ExternalOutput").ap()

    if debug:
        dbg_acc = nc.dram_tensor("dbg_acc", [NTOK, D], F32, kind="ExternalOutput").ap()
        dbg_x1 = nc.dram_tensor("dbg_x1", [NTOK, D], F32, kind="ExternalOutput").ap()
        dbg_gates = nc.dram_tensor("dbg_gates", [NTOK, 32], F32, kind="ExternalOutput").ap()
        dbg_b1c = nc.dram_tensor("dbg_b1c", [128, 512], F32, kind="ExternalOutput").ap()
        dbg_act = nc.dram_tensor("dbg_act", [128, 8 * 512], BF16, kind="ExternalOutput").ap()
        dbg_gt = nc.dram_tensor("dbg_gt", [128, 3 * 512], F32, kind="ExternalOutput").ap()
    S = Sched(nc, es)
    sb = lambda name, shape, dt=F32: es.enter_context(nc.sbuf_tensor(name, shape, dt))

    acc = sb("acc", [128, NTB, D])
    xT = sb("xT", [128, 8, NTOK], BF16)
    gates = sb("gates", [128, NTB, 32])
    b1c = sb("b1c", [128, 32, 16])
    identb = sb("identb", [128, 128], BF16)
    identf = sb("identf", [128, 128])
    eps = sb("eps", [128, 1])
    st = sb("st", [128, 2, 6])
    mv = sb("mv", [128, 2])
    rstd = sb("rstd", [128, 1])
    r_acc = [Res() for _ in range(NTB)]
    r_xT = [Res() for _ in range(NTB)]
    r_gates = [Res() for _ in range(NTB)]
    r_b1c, r_id, r_eps, r_ln = Res(), Res(), Res(), Res()
    lnt = {"st": st, "mv": mv, "rstd": rstd, "res": r_ln, "eps": eps, "eps_r": r_eps}

    S.op("pool", lambda E: E.memset(identf[:], 1.0), w=[r_id])
    S.op("pool", lambda E: E.affine_select(out=identf[:], in_=identf[:], pattern=[[-1, 128]], compare_op=ALU.is_equal, fill=0.0, base=0, channel_multiplier=1), r=[r_id], w=[r_id])
    S.op("pool", lambda E: E.tensor_copy(out=identb[:], in_=identf[:]), r=[r_id], w=[r_id])
    S.op("pool", lambda E: E.memset(eps[:], 1e-5), w=[r_eps])

    with contextlib.ExitStack() as e1:
        sb1 = lambda name, shape, dt=F32: e1.enter_context(nc.sbuf_tensor("a1_" + name, shape, dt))
        ps1 = lambda name, shape, dt=F32: e1.enter_context(nc.psum_tensor("a1_" + name, shape, dt))
        woutb = sb1("woutb", [128, 8, D], BF16)
        g1, bb1 = sb1("g1", [128, D]), sb1("bb1", [128, D])
        rwf = sb1("rwf", [128, 8, 32])
        rbb = sb1("rbb", [128, 32])
        b2all = sb1("b2all", [32, D])
        ytile = [sb1("ytile%d" % i, [128, D], BF16) for i in range(2)]
        stile = [sb1("stile%d" % i, [128, D]) for i in range(2)]
        yT = sb1("yT", [128, 8, 128], BF16)
        x1 = sb1("x1", [128, D])
        xnb = sb1("xnb", [128, D], BF16)
        xTf = sb1("xTf", [128, 8, 128])
        lg = sb1("lg", [128, 32])
        m8 = sb1("m8", [128, 8])
        nmx = sb1("nmx", [128, 1])
        msk = sb1("msk", [128, 32])
        ex = sb1("ex", [128, 32])
        den = sb1("den", [128, 1])
        gT = sb1("gT", [32, 128])
        tp = ps1("tp", [128, 8, 128], BF16)
        mx = [ps1("mx%d" % i, [128, 512]) for i in range(2)]
        tpf = ps1("tpf", [128, 4, 128])
        lgp = ps1("lgp", [128, 32])
        gtp = ps1("gtp", [32, 128])
        bt = [ps1("bt%d" % i, [128, 512]) for i in range(2)]
        r_w, r_g1, r_rw, r_b2 = Res(), Res(), Res(), Res()
        r_yt, r_st = [Res(), Res()], [Res(), Res()]
        r_yT, r_x1, r_xnb, r_xTf, r_sm, r_gT = Res(), Res(), Res(), Res(), Res(), Res()
        r_tp, r_mx, r_tpf, r_lgp, r_gtp, r_bt = Res(), [Res(), Res()], Res(), Res(), Res(), [Res(), Res()]

        for c in range(8):
            S.dma("pool", woutb[:, c, :], wout[c * 128:(c + 1) * 128, :], w=[r_w])
        S.dma("sp", g1[:], ln1g.partition_broadcast(128), w=[r_g1])
        S.dma("sp", bb1[:], ln1b.partition_broadcast(128), w=[r_g1])
        S.dma("sp", rwf[:], rw.rearrange("(c p) e -> p c e", p=128), w=[r_rw])
        S.dma("sp", rbb[:], rbias.partition_broadcast(128), w=[r_rw])
        S.dma("sp", b2all[:], b2[:, :], w=[r_b2])

        b1raw = sb1("b1raw", [32, 2 * D])
        r_b1raw = Res()
        S.dma("sp", b1raw[:], b1[:, :], w=[r_b1raw])
        for h in range(16):
            S.op("pe", lambda E, h=h: E.transpose(out=lgp[:], in_=b1raw[:, h * 128:(h + 1) * 128], identity=identf[0:32, 0:32]), r=[r_b1raw, r_id], w=[r_lgp])
            S.op("act", lambda E, h=h: E.copy(out=b1c[:, :, h], in_=lgp[:]), r=[r_lgp], w=[r_b1c])

        for T in range(NTB):
            yt, st_ = ytile[T % 2], stile[T % 2]
            S.dma("sp", yt[:], yg[T * 128:(T + 1) * 128, :], w=[r_yt[T % 2]])
            S.dma("sp", st_[:], s32[T * 128:(T + 1) * 128, :], w=[r_st[T % 2]])
            for c in range(8):
                S.op("pe", lambda E, c=c: E.transpose(out=tp[:, c, :], in_=yt[:, c * 128:(c + 1) * 128], identity=identb[:]), r=[r_yt[T % 2], r_id], w=[r_tp])
            S.op("act", lambda E: E.copy(out=yT[:], in_=tp[:]), r=[r_tp], w=[r_yT])
            for h in range(2):
                for c in range(8):
                    S.op("pe", lambda E, h=h, c=c: E.matmul(mx[h][:], lhsT=yT[:, c, :], rhs=woutb[:, c, h * 512:(h + 1) * 512], start=(c == 0), stop=(c == 7)), r=[r_yT, r_w], w=[r_mx[h]])
                S.op("dve", lambda E, h=h: E.scalar_tensor_tensor(out=x1[:, h * 512:(h + 1) * 512], in0=st_[:, h * 512:(h + 1) * 512], scalar=ALPHA, in1=mx[h][:], op0=ALU.mult, op1=ALU.add), r=[r_st[T % 2], r_mx[h]], w=[r_x1])
            if debug:
                S.dma("sp", dbg_x1[T * 128:(T + 1) * 128, :], x1[:], r=[r_x1], is_out=True)
            _layernorm_tile(S, lnt, x1[:], acc[:, T, :], g1, bb1, [r_x1], [r_acc[T]], r_g1)
            S.op("act", lambda E: E.copy(out=xnb[:], in_=acc[:, T, :]), r=[r_acc[T]], w=[r_xnb])
            for c in range(8):
                S.op("pe", lambda E, c=c: E.transpose(out=tp[:, c, :], in_=xnb[:, c * 128:(c + 1) * 128], identity=identb[:]), r=[r_xnb, r_id], w=[r_tp])
            S.op("act", lambda E: E.copy(out=xT[:, :, T * 128:(T + 1) * 128], in_=tp[:]), r=[r_tp], w=[r_xT[T]])
            for q in range(2):
                for c in range(4):
                    S.op("pe", lambda E, q=q, c=c: E.transpose(out=tpf[:, c, :], in_=acc[:, T, (q * 4 + c) * 128:(q * 4 + c + 1) * 128], identity=identf[:]), r=[r_acc[T], r_id], w=[r_tpf])
                S.op("act", lambda E, q=q: E.copy(out=xTf[:, q * 4:(q + 1) * 4, :], in_=tpf[:]), r=[r_tpf], w=[r_xTf])
            for c in range(8):
                S.op("pe", lambda E, c=c: E.matmul(lgp[:], lhsT=xTf[:, c, :], rhs=rwf[:, c, :], start=(c == 0), stop=(c == 7)), r=[r_xTf, r_rw], w=[r_lgp])
            S.op("dve", lambda E: E.tensor_tensor(out=lg[:], in0=lgp[:], in1=rbb[:], op=ALU.add), r=[r_lgp, r_rw], w=[r_sm])
            S.op("dve", lambda E: E.max(out=m8[:], in_=lg[:]), r=[r_sm], w=[r_sm])
            S.op("dve", lambda E: E.tensor_scalar(out=nmx[:], in0=m8[:, 0:1], scalar1=-1.0, scalar2=None, op0=ALU.mult), r=[r_sm], w=[r_sm])
            S.op("dve", lambda E: E.tensor_scalar(out=msk[:], in0=lg[:], scalar1=m8[:, 3:4], scalar2=None, op0=ALU.is_ge), r=[r_sm], w=[r_sm])
            S.op("act", lambda E: E.activation(out=ex[:], in_=lg[:], func=AF.Exp, bias=nmx[:], scale=1.0), r=[r_sm], w=[r_sm])
            S.op("dve", lambda E: E.tensor_tensor(out=ex[:], in0=ex[:], in1=msk[:], op=ALU.mult), r=[r_sm], w=[r_sm])
            S.op("dve", lambda E: E.tensor_reduce(out=den[:], in_=ex[:], axis=AX.X, op=ALU.add), r=[r_sm], w=[r_sm])
            S.op("dve", lambda E: E.reciprocal(out=den[:], in_=den[:]), r=[r_sm], w=[r_sm])
            S.op("dve", lambda E: E.tensor_scalar(out=gates[:, T, :], in0=ex[:], scalar1=den[:], scalar2=None, op0=ALU.mult), r=[r_sm], w=[r_gates[T]])
            S.op("pe", lambda E: E.transpose(out=gtp[:], in_=gates[:, T, :], identity=identf[:]), r=[r_gates[T], r_id], w=[r_gtp])
            S.op("act", lambda E: E.copy(out=gT[:], in_=gtp[:]), r=[r_gtp], w=[r_gT])
            for h in range(2):
                S.op("pe", lambda E, h=h: E.matmul(bt[h][:], lhsT=gT[:], rhs=b2all[:, h * 512:(h + 1) * 512], start=True, stop=True), r=[r_gT, r_b2], w=[r_bt[h]])
                S.op("dve", lambda E, h=h: E.scalar_tensor_tensor(out=acc[:, T, h * 512:(h + 1) * 512], in0=acc[:, T, h * 512:(h + 1) * 512], scalar=ALPHA, in1=bt[h][:], op0=ALU.mult, op1=ALU.add), r=[r_bt[h], r_xnb, r_tpf], w=[r_acc[T]])

    S.barrier()
    if debug:
        for T in range(NTB):
            S.dma("sp", dbg_acc[T * 128:(T + 1) * 128, :], acc[:, T, :], r=[r_acc[T]], is_out=True)
            S.dma("sp", dbg_gates[T * 128:(T + 1) * 128, :], gates[:, T, :], r=[r_gates[T]], is_out=True)
    groups = [(g * 512, min(512, NTOK - g * 512)) for g in range((NTOK + 511) // 512)]
    with contextlib.ExitStack() as e2:
        sb2 = lambda name, shape, dt=F32: e2.enter_context(nc.sbuf_tensor("a2_" + name, shape, dt))
        ps2 = lambda name, shape, dt=F32: e2.enter_context(nc.psum_tensor("a2_" + name, shape, dt))
        w1b = [sb2("w1b%d" % i, [128, 8, 2 * D], BF16) for i in range(2)]
        w2b = sb2("w2b", [128, 8, D], BF16)
        actT = sb2("actT", [128, 8, 512], BF16)
        gt = sb2("gt", [128, 512])
        sg = sb2("sg", [128, 512])
        lt = sb2("lt", [128, 512])
        pg = [ps2("pg%d" % i, [128, 512]) for i in range(2)]
        pl = [ps2("pl%d" % i, [128, 512]) for i in range(2)]
        py = [ps2("py%d" % i, [128, 512]) for i in range(4)]
        r_w1 = [[Res() for _ in range(8)] for _ in range(2)]
        r_w2 = Res()
        r_actT = [Res() for _ in range(8)]
        r_gt, r_sg, r_lt = Res(), Res(), Res()
        r_pg, r_pl, r_py = [Res(), Res()], [Res(), Res()], [Res() for _ in range(4)]
        ipy = 0
        ih = 0

        def load_w1(e):
            for c in range(8):
                S.dma("pool", w1b[e % 2][:, c, :], w1[e, c * 128:(c + 1) * 128, :], w=[r_w1[e % 2][c]])

        if ne > 0:
            load_w1(0)
        for e in range(ne):
            for c in range(8):
                S.dma("pool", w2b[:, c, :], w2[e, c * 128:(c + 1) * 128, :], w=[r_w2])
            if e + 1 < ne:
                load_w1(e + 1)
            wb = w1b[e % 2]
            for (t0, N) in groups:
                for h in range(8):
                    k = ih % 2
                    ih += 1
                    for c in range(8):
                        S.op("pe", lambda E, c=c, h=h, k=k: E.matmul(pg[k][:, 0:N], lhsT=wb[:, c, h * 128:(h + 1) * 128], rhs=xT[:, c, t0:t0 + N], start=(c == 0), stop=(c == 7)), r=[r_w1[e % 2][c]] + r_xT[t0 // 128:(t0 + N) // 128], w=[r_pg[k]])
                    for c in range(8):
                        S.op("pe", lambda E, c=c, h=h, k=k: E.matmul(pl[k][:, 0:N], lhsT=wb[:, c, D + h * 128:D + (h + 1) * 128], rhs=xT[:, c, t0:t0 + N], start=(c == 0), stop=(c == 7)), r=[r_w1[e % 2][c]] + r_xT[t0 // 128:(t0 + N) // 128], w=[r_pl[k]])
                    S.op("dve", lambda E, h=h, k=k: E.tensor_scalar(out=gt[:, 0:N], in0=pg[k][:, 0:N], scalar1=b1c[:, e, h:h + 1], scalar2=7.0, op0=ALU.add, op1=ALU.min), r=[r_pg[k], r_b1c], w=[r_gt])
                    S.op("act", lambda E: E.activation(out=sg[:, 0:N], in_=gt[:, 0:N], func=AF.Sigmoid, scale=1.702), r=[r_gt], w=[r_sg])
                    S.op("dve", lambda E, h=h, k=k: E.tensor_scalar(out=lt[:, 0:N], in0=pl[k][:, 0:N], scalar1=b1c[:, e, 8 + h:9 + h], scalar2=7.0, op0=ALU.add, op1=ALU.min), r=[r_pl[k], r_b1c], w=[r_lt])
                    S.op("dve", lambda E: E.tensor_scalar(out=lt[:, 0:N], in0=lt[:, 0:N], scalar1=-7.0, scalar2=1.0, op0=ALU.max, op1=ALU.add), r=[r_lt], w=[r_lt])
                    S.op("dve", lambda E: E.tensor_tensor(out=gt[:, 0:N], in0=gt[:, 0:N], in1=sg[:, 0:N], op=ALU.mult), r=[r_sg], w=[r_gt])
                    S.op("dve", lambda E, h=h: E.tensor_tensor(out=actT[:, h, 0:N], in0=gt[:, 0:N], in1=lt[:, 0:N], op=ALU.mult), r=[r_gt, r_lt], w=[r_actT[h]])
                for s in range(N // 128):
                    T = t0 // 128 + s
                    for hf in range(2):
                        k = ipy % 4
                        ipy += 1
                        for h in range(8):
                            S.op("pe", lambda E, h=h, k=k, s=s, hf=hf: E.matmul(py[k][:], lhsT=actT[:, h, s * 128:(s + 1) * 128], rhs=w2b[:, h, hf * 512:(hf + 1) * 512], start=(h == 0), stop=(h == 7)), r=[r_actT[h], r_w2], w=[r_py[k]])
                        S.op("dve", lambda E, k=k, hf=hf, T=T: E.scalar_tensor_tensor(out=acc[:, T, hf * 512:(hf + 1) * 512], in0=py[k][:], scalar=gates[:, T, e:e + 1], in1=acc[:, T, hf * 512:(hf + 1) * 512], op0=ALU.mult, op1=ALU.add), r=[r_py[k], r_gates[T]], w=[r_acc[T]])

        if debug and ne > 0:
            S.dma("sp", dbg_b1c[:, :], b1c[:].rearrange("p a b -> p (a b)"), r=[r_b1c], is_out=True)
            S.dma("sp", dbg_act[:, :], actT[:].rearrange("p a b -> p (a b)"), r=r_actT, is_out=True)
            S.dma("sp", dbg_gt[:, 0:512], gt[:], r=[r_gt], is_out=True)
            S.dma("sp", dbg_gt[:, 512:1024], sg[:], r=[r_sg], is_out=True)
            S.dma("sp", dbg_gt[:, 1024:1536], lt[:], r=[r_lt], is_out=True)
    S.barrier()
    with contextlib.ExitStack() as e3:
        sb3 = lambda name, shape, dt=F32: e3.enter_context(nc.sbuf_tensor(name, shape, dt))
        g2, bb2 = sb3("g2", [128, D]), sb3("bb2", [128, D])
        ot = [sb3("ot%d" % i, [128, D]) for i in range(2)]
        ob = [sb3("ob%d" % i, [128, D], BF16) for i in range(2)]
        r_g2 = Res()
        r_ot, r_ob = [Res(), Res()], [Res(), Res()]
        S.dma("sp", g2[:], ln2g.partition_broadcast(128), w=[r_g2])
        S.dma("sp", bb2[:], ln2b.partition_broadcast(128), w=[r_g2])
        for T in range(NTB):
            k = T % 2
            _layernorm_tile(S, lnt, acc[:, T, :], ot[k][:], g2, bb2, [r_acc[T]], [r_ot[k]], r_g2)
            S.op("act", lambda E, k=k: E.copy(out=ob[k][:], in_=ot[k][:]), r=[r_ot[k]], w=[r_ob[k]])
            S.dma("sp", out32[T * 128:(T + 1) * 128, :], ot[k][:], r=[r_ot[k]], is_out=True)
            S.dma("sp", outbf[T * 128:(T + 1) * 128, :], ob[k][:], r=[r_ob[k]], is_out=True)
    S.finish()
    return nc


WC = 1024
C_DQ, C_DK, C_DV, C_FQ, C_FK, C_FV, C_GQ, C_GZ = 0, 128, 256, 384, 448, 576, 704, 896
P_LAM, P_DSG, P_FFB, P_ALOG, P_DTB, P_GNG, P_GCW, PRM = 0, 256, 384, 385, 386, 448, 512, 1280


def build_A(first, lam_init, stages=(1, 2, 3)):
    nc = bass.Bass("TRN2", target_bir_lowering=False)
    es = contextlib.ExitStack()

    def din(name, shape, dt=F32):
        return nc.dram_tensor(name, shape, dt, kind="ExternalInput").ap()

    if first:
        x = din("x", [8192, D])
        meta = din("meta", [16, D])
        lng, lnb = din("lng", [1, D]), din("lnb", [1, D])
        s32o = nc.dram_tensor("s32o", [L, D], F32, kind="ExternalOutput").ap()
    else:
        sbf = din("sbf", [L, D], BF16)
    w = din("w", [D, WC])
    prm = din("prm", [1, PRM])
    y = nc.dram_tensor("y", [L, 256], BF16, kind="ExternalOutput").ap()
    xTd = nc.dram_tensor("xTd", [8, 128, L], BF16).ap()

    S = Sched(nc, es)
    sb = lambda name, shape, dt=F32: es.enter_context(nc.sbuf_tensor(name, shape, dt))
    chunks = [(g * 512, min(512, L - g * 512)) for g in range((L + 511) // 512)]

    wbf = sb("wbf", [128, 8, WC], BF16)
    ybuf = sb("ybuf", [128, NTL, 256], BF16)
    identb = sb("identb", [128, 128], BF16)
    identf = sb("identf", [128, 128])
    trif = sb("trif", [128, 128])
    onesf = sb("onesf", [128, 128])
    cmaskb = sb("cmaskb", [128, 128], BF16)
    dmaskb = sb("dmaskb", [128, 128], BF16)
    smask = sb("smask", [128, 128])
    pmask = sb("pmask", [128, 1])
    c_eps5, c_eps6, c_one = sb("c_eps5", [128, 1]), sb("c_eps6", [128, 1]), sb("c_one", [128, 1])
    prow = sb("prow", [1, PRM])
    pcol = sb("pcol", [128, 8])
    r_w, r_y, r_c, r_xTd, r_prow, r_pcol = Res(), [Res() for _ in range(NTL)], Res(), Res(), Res(), Res()

    def aff(out, base, cm, step, op=ALU.is_ge):
        S.op("pool", lambda E: E.memset(out, 1.0), w=[r_c])
        S.op("pool", lambda E: E.affine_select(out=out, in_=out, pattern=[[step, 128]], compare_op=op, fill=0.0, base=base, channel_multiplier=cm), r=[r_c], w=[r_c])

    aff(identf[:], 0, 1, -1, ALU.is_equal)
    aff(trif[:], 0, -1, 1)
    aff(smask[:], -1, 1, -1)
    S.op("pool", lambda E: E.tensor_copy(out=identb[:], in_=identf[:]), r=[r_c], w=[r_c])
    S.op("pool", lambda E: E.tensor_copy(out=cmaskb[:], in_=trif[:]), r=[r_c], w=[r_c])
    S.op("pool", lambda E: E.memset(onesf[:], 1.0), w=[r_c])
    S.op("pool", lambda E: E.memset(dmaskb[:], 1.0), w=[r_c])
    S.op("pool", lambda E: E.memset(dmaskb[64:128, 0:64], 0.0), r=[r_c], w=[r_c])
    S.op("pool", lambda E: E.memset(pmask[:], 1.0), w=[r_c])
    S.op("pool", lambda E: E.memset(pmask[0:112, :], 0.0), r=[r_c], w=[r_c])
    S.op("pool", lambda E: E.memset(c_eps5[:], 1e-5), w=[r_c])
    S.op("pool", lambda E: E.memset(c_eps6[:], 1e-6), w=[r_c])
    S.op("pool", lambda E: E.memset(c_one[:], 1.0), w=[r_c])
    for c in range(8):
        S.dma("pool", wbf[:, c, :], w[c * 128:(c + 1) * 128, :], w=[r_w])
    S.dma("sp", prow[:], prm[:, :], w=[r_prow])

    with contextlib.ExitStack() as e0:
        sb0 = lambda name, shape, dt=F32: e0.enter_context(nc.sbuf_tensor(name, shape, dt))
        tp = e0.enter_context(nc.psum_tensor("tp0", [128, 8, 128], BF16))
        pbc = e0.enter_context(nc.psum_tensor("pbc", [128, 8]))
        r_tp, r_pbc = Res(), Res()
        xin = [sb0("xin%d" % i, [128, D], F32 if first else BF16) for i in range(2)]
        xb = [sb0("xb%d" % i, [128, D], BF16) for i in range(2)]
        xTt = [sb0("xTt%d" % i, [128, 8, 128], BF16) for i in range(2)]
        r_xin, r_xb, r_xTt = [Res(), Res()], [Res(), Res()], [Res(), Res()]
        srow = sb0("srow", [1, 8])
        tmp64 = sb0("tmp64", [1, 128])
        r_srow = Res()
        S.op("dve", lambda E: E.memset(srow[:], 0.0), w=[r_srow])
        S.op("dve", lambda E: E.tensor_tensor(out=tmp64[:, 0:64], in0=prow[:, P_LAM:P_LAM + 64], in1=prow[:, P_LAM + 64:P_LAM + 128], op=ALU.mult), r=[r_prow], w=[r_srow])
        S.op("dve", lambda E: E.tensor_tensor(out=tmp64[:, 64:128], in0=prow[:, P_LAM + 128:P_LAM + 192], in1=prow[:, P_LAM + 192:P_LAM + 256], op=ALU.mult), r=[r_prow], w=[r_srow])
        S.op("dve", lambda E: E.tensor_reduce(out=srow[:, 4:6], in_=tmp64[:].rearrange("p (a b) -> p a b", a=2), axis=AX.X, op=ALU.add), r=[r_srow], w=[r_srow])
        S.op("act", lambda E: E.activation(out=srow[:, 4:6], in_=srow[:, 4:6], func=AF.Exp), r=[r_srow], w=[r_srow])
        S.op("dve", lambda E: E.tensor_tensor(out=srow[:, 3:4], in0=srow[:, 5:6], in1=srow[:, 4:5], op=ALU.subtract), r=[r_srow], w=[r_srow])
        S.op("dve", lambda E: E.tensor_scalar(out=srow[:, 3:4], in0=srow[:, 3:4], scalar1=-float(lam_init), scalar2=None, op0=ALU.add), r=[r_srow], w=[r_srow])
        S.op("dve", lambda E: E.tensor_scalar(out=srow[:, 0:1], in0=prow[:, P_FFB:P_FFB + 1], scalar1=-1.0, scalar2=None, op0=ALU.mult), r=[r_prow, r_srow], w=[r_srow])
        S.op("dve", lambda E: E.tensor_copy(out=srow[:, 1:2], in_=prow[:, P_DTB:P_DTB + 1]), r=[r_prow, r_srow], w=[r_srow])
        S.op("act", lambda E: E.activation(out=srow[:, 2:3], in_=prow[:, P_ALOG:P_ALOG + 1], func=AF.Exp), r=[r_prow, r_srow], w=[r_srow])
        S.op("dve", lambda E: E.tensor_scalar(out=srow[:, 2:3], in0=srow[:, 2:3], scalar1=-1.0, scalar2=None, op0=ALU.mult), r=[r_srow], w=[r_srow])
        S.op("pe", lambda E: E.matmul(pbc[:], lhsT=onesf[0:1, :], rhs=srow[:], start=True, stop=True), r=[r_srow, r_c], w=[r_pbc])
        S.op("act", lambda E: E.copy(out=pcol[:], in_=pbc[:]), r=[r_pbc], w=[r_pcol])
        if first:
            gb_, bb_ = sb0("lngb", [128, D]), sb0("lnbb", [128, D])
            st, mv, rstd = sb0("st", [128, 2, 6]), sb0("mv", [128, 2]), sb0("rstd", [128, 1])
            xn = [sb0("xn%d" % i, [128, D]) for i in range(2)]
            r_xn, r_g, r_ln = [Res(), Res()], Res(), Res()
            lnt = {"st": st, "mv": mv, "rstd": rstd, "res": r_ln, "eps": c_eps5, "eps_r": r_c}
            S.dma("sp", gb_[:], lng.partition_broadcast(128), w=[r_g])
            S.dma("sp", bb_[:], lnb.partition_broadcast(128), w=[r_g])
        for T in range(NTL):
            k = T % 2
            if first:
                if T == 0:
                    S.op("pool", lambda E: E.memset(xin[0][:], 0.0), w=[r_xin[0]])
                    S.dma("sp", xin[0][112:128, :], meta[:, :], w=[r_xin[0]])
                else:
                    S.dma("sp", xin[k][:], x[(T - 1) * 128:T * 128, :], w=[r_xin[k]])
                _layernorm_tile(S, lnt, xin[k][:], xn[k][:], gb_, bb_, [r_xin[k]], [r_xn[k]], r_g)
                S.dma("sp", s32o[T * 128:(T + 1) * 128, :], xn[k][:], r=[r_xn[k]], is_out=True)
                src, rsrc = xn[k], r_xn[k]
            else:
                S.dma("sp", xin[k][:], sbf[T * 128:(T + 1) * 128, :], w=[r_xin[k]])
                src, rsrc = xin[k], r_xin[k]
            S.op("act", lambda E: E.copy(out=xb[k][:], in_=src[:]), r=[rsrc], w=[r_xb[k]])
            if T == 0:
                S.op("pool", lambda E: E.memset(xb[k][0:112, :], 0.0), r=[r_xb[k]], w=[r_xb[k]])
            for c in range(8):
                S.op("pe", lambda E, c=c: E.transpose(out=tp[:, c, :], in_=xb[k][:, c * 128:(c + 1) * 128], identity=identb[:]), r=[r_xb[k], r_c], w=[r_tp])
            S.op("dve", lambda E: E.tensor_copy(out=xTt[k][:], in_=tp[:]), r=[r_tp], w=[r_xTt[k]])
            S.dma("sp", xTd[:, :, T * 128:(T + 1) * 128].rearrange("c p t -> p c t"), xTt[k][:], r=[r_xTt[k]], w=[r_xTd])
    S.barrier()

    def attention(kT, qT, base, vaug, dvp1, G, maskb, btab, r_in, handler, psS, psO, pT, r_psS, r_psO, r_pT):
        ngroups = (NTL + G - 1) // G
        items = []
        for g in range(ngroups):
            gb0 = g * G
            Gc = min(G, NTL - gb0)
            for kb in range(gb0 + Gc):
                items.append((g, gb0, Gc, kb))
        n = len(items)

        def emit_qk(it):
            g, gb0, Gc, kb = items[it]
            smin = max(0, kb - gb0)
            ncol = (Gc - smin) * 128
            i3 = it % 3
            Sp, Pt = psS[i3], pT[i3]
            S.op("pe", lambda E: E.matmul(Sp[:, 0:ncol], lhsT=kT[base:base + 64, kb * 128:(kb + 1) * 128], rhs=qT[base:base + 64, (gb0 + smin) * 128:(gb0 + Gc) * 128], start=True, stop=True), r=r_in, w=[r_psS[i3]])
            if btab is None:
                S.op("act", lambda E: E.activation(out=Pt[:, 0:ncol], in_=Sp[:, 0:ncol], func=AF.Exp), r=[r_psS[i3]], w=[r_pT[i3]])
            else:
                for s in range(smin, Gc):
                    cs = (s - smin) * 128
                    S.op("act", lambda E, s=s, cs=cs: E.activation(out=Pt[:, cs:cs + 128], in_=Sp[:, cs:cs + 128], func=AF.Exp, bias=btab[:, kb, gb0 + s:gb0 + s + 1], scale=1.0), r=[r_psS[i3]] + r_in, w=[r_pT[i3]])
            if kb >= gb0:
                S.op("pool", lambda E: E.tensor_tensor(out=Pt[:, 0:128], in0=Pt[:, 0:128], in1=maskb[:], op=ALU.mult), r=[r_c], w=[r_pT[i3]])

        def emit_pv(it):
            g, gb0, Gc, kb = items[it]
            smin = max(0, kb - gb0)
            i3 = it % 3
            Pt = pT[i3]
            Oi, r_Oi = psO[g % 2], r_psO[g % 2]
            for s in range(smin, Gc):
                cs = (s - smin) * 128
                S.op("pe", lambda E, s=s, cs=cs: E.matmul(Oi[:, s, 0:dvp1], lhsT=Pt[:, cs:cs + 128], rhs=vaug[:, kb, 0:dvp1], start=(kb == 0 and s == 0), stop=(kb == gb0 + s), skip_group_check=True), r=[r_pT[i3]] + r_in, w=[r_Oi])
            if kb == gb0 + Gc - 1:
                handler(gb0, Gc, Oi, r_Oi)

        for it in range(n + 1):
            if it < n:
                emit_qk(it)
            if it >= 1:
                emit_pv(it - 1)

    def load_chunk(xTc, r_x, t0, N, halo=0):
        if halo and t0 == 0:
            S.op("pool", lambda E: E.memset(xTc[:, :, 0:halo], 0.0), w=[r_x])
            S.dma("sp", xTc[:, :, halo:halo + N], xTd[:, :, 0:N].rearrange("c p t -> p c t"), r=[r_xTd], w=[r_x])
        else:
            S.dma("sp", xTc[:, :, 0:N + halo], xTd[:, :, t0 - halo:t0 + N].rearrange("c p t -> p c t"), r=[r_xTd], w=[r_x])

    if 1 in stages:
      with contextlib.ExitStack() as e1:
        sb1 = lambda name, shape, dt=F32: e1.enter_context(nc.sbuf_tensor("a1_" + name, shape, dt))
        ps1 = lambda name, shape, dt=F32: e1.enter_context(nc.psum_tensor("a1_" + name, shape, dt))
        dqT, dkT = sb1("dqT", [128, L], BF16), sb1("dkT", [128, L], BF16)
        dva = sb1("dva", [128, NTL, 129], BF16)
        O0 = sb1("O0", [128, NTL, 129])
        xTc = [sb1("xTc%d" % i, [128, 8, 512], BF16) for i in range(2)]
        pT = [sb1("pT%d" % i, [128, 512], BF16) for i in range(3)]
        gsub = sb1("gsub", [128, 128])
        rd = sb1("rd", [128, 8])
        t1 = sb1("t1", [128, 128])
        ot = sb1("ot", [128, 128])
        junk = sb1("junk", [128, 128])
        ss = sb1("ss", [128, 1])
        psS = [ps1("psS%d" % i, [128, 512]) for i in range(3)]
        psO = [ps1("psO%d" % i, [128, 3, 129]) for i in range(2)]
        pq, pk, pv = ps1("pq", [128, 512]), ps1("pk", [128, 512]), ps1("pv", [128, 512])
        r_in, r_xc, r_pT, r_psS, r_psO = Res(), [Res(), Res()], [Res() for _ in range(3)], [Res() for _ in range(3)], [Res(), Res()]
        r_pq, r_pk, r_pv, r_O0, r_gs, r_h = Res(), Res(), Res(), Res(), Res(), Res()
        S.dma("sp", gsub[:], prm[:, P_DSG:P_DSG + 128].partition_broadcast(128), w=[r_gs])
        S.op("act", lambda E: E.mul(gsub[:], gsub[:], float(1.0 - lam_init)), r=[r_gs], w=[r_gs])
        S.op("pool", lambda E: E.memset(dva[:, :, 128:129], 1.0), w=[r_in])
        for ci, (t0, N) in enumerate(chunks):
            xc, r_x = xTc[ci % 2], r_xc[ci % 2]
            load_chunk(xc, r_x, t0, N)
            for c in range(8):
                S.op("pe", lambda E, c=c: E.matmul(pq[:, 0:N], lhsT=wbf[:, c, C_DQ:C_DQ + 128], rhs=xc[:, c, 0:N], start=(c == 0), stop=(c == 7)), r=[r_w, r_x], w=[r_pq])
            S.op("act", lambda E: E.mul(dqT[:, t0:t0 + N], pq[:, 0:N], 0.125), r=[r_pq], w=[r_in])
            for c in range(8):
                S.op("pe", lambda E, c=c: E.matmul(pk[:, 0:N], lhsT=wbf[:, c, C_DK:C_DK + 128], rhs=xc[:, c, 0:N], start=(c == 0), stop=(c == 7)), r=[r_w, r_x], w=[r_pk])
            S.op("dve", lambda E: E.tensor_copy(out=dkT[:, t0:t0 + N], in_=pk[:, 0:N]), r=[r_pk], w=[r_in])
            for s in range(N // 128):
                T = t0 // 128 + s
                for c in range(8):
                    S.op("pe", lambda E, c=c, s=s: E.matmul(pv[:, 0:128], lhsT=xc[:, c, s * 128:(s + 1) * 128], rhs=wbf[:, c, C_DV:C_DV + 128], start=(c == 0), stop=(c == 7)), r=[r_w, r_x], w=[r_pv])
                S.op("act", lambda E, T=T: E.copy(out=dva[:, T, 0:128], in_=pv[:, 0:128]), r=[r_pv], w=[r_in])
        S.op("pool", lambda E: E.memset(dva[0:112, 0, :], 0.0), r=[r_in], w=[r_in])

        def h0(gb0, Gc, Oi, r_Oi):
            S.op("act", lambda E: E.copy(out=O0[:, gb0:gb0 + Gc, :], in_=Oi[:, 0:Gc, :]), r=[r_Oi], w=[r_O0])

        def h1(gb0, Gc, Oi, r_Oi):
            S.op("dve", lambda E: E.reciprocal(out=rd[:, 0:Gc], in_=O0[:, gb0:gb0 + Gc, 128]), r=[r_O0], w=[r_h])
            S.op("dve", lambda E: E.reciprocal(out=rd[:, 4:4 + Gc], in_=Oi[:, 0:Gc, 128]), r=[r_Oi, r_h], w=[r_h])
            S.op("dve", lambda E: E.tensor_scalar(out=rd[:, 4:4 + Gc], in0=rd[:, 4:4 + Gc], scalar1=pcol[:, 3:4], scalar2=None, op0=ALU.mult), r=[r_h, r_pcol], w=[r_h])
            for s in range(Gc):
                T = gb0 + s
                S.op("dve", lambda E, s=s: E.tensor_scalar(out=t1[:], in0=Oi[:, s, 0:128], scalar1=rd[:, 4 + s:5 + s], scalar2=None, op0=ALU.mult), r=[r_Oi, r_h], w=[r_h])
                S.op("dve", lambda E, s=s, T=T: E.scalar_tensor_tensor(out=ot[:], in0=O0[:, T, 0:128], scalar=rd[:, s:s + 1], in1=t1[:], op0=ALU.mult, op1=ALU.add), r=[r_O0, r_h], w=[r_h])
                S.op("act", lambda E: E.activation(out=junk[:], in_=ot[:], func=AF.Square, accum_out=ss[:]), r=[r_h], w=[r_h])
                S.op("act", lambda E: E.activation(out=ss[:], in_=ss[:], func=AF.Sqrt, bias=c_eps5[:], scale=1.0 / 128), r=[r_h, r_c], w=[r_h])
                S.op("dve", lambda E: E.reciprocal(out=ss[:], in_=ss[:]), r=[r_h], w=[r_h])
                S.op("dve", lambda E, T=T: E.scalar_tensor_tensor(out=ybuf[:, T, 0:128], in0=ot[:], scalar=ss[:], in1=gsub[:], op0=ALU.mult, op1=ALU.mult), r=[r_h, r_gs], w=[r_y[T]])

        attention(dkT, dqT, 0, dva, 129, 3, dmaskb, None, [r_in], h0, psS, psO, pT, r_psS, r_psO, r_pT)
        attention(dkT, dqT, 64, dva, 129, 3, dmaskb, None, [r_in], h1, psS, psO, pT, r_psS, r_psO, r_pT)
      S.barrier()

    if 2 in stages:
      with contextlib.ExitStack() as e2:
        sb2 = lambda name, shape, dt=F32: e2.enter_context(nc.sbuf_tensor("a2_" + name, shape, dt))
        ps2 = lambda name, shape, dt=F32: e2.enter_context(nc.psum_tensor("a2_" + name, shape, dt))
        fqT, fkT = sb2("fqT", [64, L], BF16), sb2("fkT", [64, L], BF16)
        fva = sb2("fva", [128, NTL, 65], BF16)
        ffl = sb2("ffl", [128, NTL])
        Cn = sb2("Cn", [128, NTL])
        tots = sb2("tots", [128, NTL])
        excl = sb2("excl", [128, NTL])
        onesr = sb2("onesr", [128, NTL])
        btab = sb2("btab", [128, NTL, NTL])
        xTc = [sb2("xTc%d" % i, [128, 8, 512], BF16) for i in range(2)]
        pT = [sb2("pT%d" % i, [128, 512], BF16) for i in range(3)]
        rd = sb2("rd", [128, 4])
        psS = [ps2("psS%d" % i, [128, 512]) for i in range(3)]
        psO = [ps2("psO%d" % i, [128, 4, 128]) for i in range(2)]
        pq, pk, pv = ps2("pq", [128, 512]), ps2("pk", [128, 512]), ps2("pv", [128, 512])
        r_in, r_xc, r_pT, r_psS, r_psO = Res(), [Res(), Res()], [Res() for _ in range(3)], [Res() for _ in range(3)], [Res(), Res()]
        r_pq, r_pk, r_pv, r_f, r_h = Res(), Res(), Res(), Res(), Res()
        S.op("pool", lambda E: E.memset(fva[:, :, 64:65], 1.0), w=[r_in])
        S.op("pool", lambda E: E.memset(onesr[:], 1.0), w=[r_f])
        for ci, (t0, N) in enumerate(chunks):
            xc, r_x = xTc[ci % 2], r_xc[ci % 2]
            load_chunk(xc, r_x, t0, N)
            for c in range(8):
                S.op("pe", lambda E, c=c: E.matmul(pq[:, 0:N], lhsT=wbf[:, c, C_FQ:C_FQ + 128], rhs=xc[:, c, 0:N], start=(c == 0), stop=(c == 7)), r=[r_w, r_x], w=[r_pq])
            S.op("act", lambda E: E.mul(fqT[:, t0:t0 + N], pq[0:64, 0:N], 0.125), r=[r_pq], w=[r_in])
            for c in range(8):
                S.op("pe", lambda E, c=c: E.matmul(pk[:, 0:N], lhsT=wbf[:, c, C_FK:C_FK + 128], rhs=xc[:, c, 0:N], start=(c == 0), stop=(c == 7)), r=[r_w, r_x], w=[r_pk])
            S.op("dve", lambda E: E.tensor_copy(out=fkT[:, t0:t0 + N], in_=pk[0:64, 0:N]), r=[r_pk], w=[r_in])
            for s in range(N // 128):
                T = t0 // 128 + s
                for c in range(8):
                    S.op("pe", lambda E, c=c, s=s: E.matmul(pv[:, 0:65], lhsT=xc[:, c, s * 128:(s + 1) * 128], rhs=wbf[:, c, C_FV:C_FV + 65], start=(c == 0), stop=(c == 7)), r=[r_w, r_x], w=[r_pv])
                S.op("act", lambda E, T=T: E.copy(out=fva[:, T, 0:64], in_=pv[:, 0:64]), r=[r_pv], w=[r_in])
                S.op("dve", lambda E, T=T: E.tensor_copy(out=ffl[:, T:T + 1], in_=pv[:, 64:65]), r=[r_pv], w=[r_f])
        S.op("pool", lambda E: E.memset(fva[0:112, 0, :], 0.0), r=[r_in], w=[r_in])
        if os.environ.get("FOXDBG") == "4":
            raise_skip = True
        else:
            raise_skip = False
        if not raise_skip:
            S.op("act", lambda E: E.activation(out=ffl[:], in_=ffl[:], func=AF.Exp, bias=pcol[:, 0:1], scale=-1.0), r=[r_f, r_pcol], w=[r_f])
            S.op("act", lambda E: E.activation(out=ffl[:], in_=ffl[:], func=AF.Ln, bias=c_one[:], scale=1.0), r=[r_f, r_c], w=[r_f])
            S.op("pe", lambda E: E.matmul(pq[:, 0:NTL], lhsT=trif[:], rhs=ffl[:], start=True, stop=True), r=[r_f, r_c], w=[r_pq])
            S.op("pe", lambda E: E.matmul(pk[:, 0:NTL], lhsT=onesf[:], rhs=ffl[:], start=True, stop=True), r=[r_f, r_c], w=[r_pk])
            S.op("dve", lambda E: E.tensor_copy(out=tots[:], in_=pk[:, 0:NTL]), r=[r_pk], w=[r_f])

            if os.environ.get("FOXDBG") == "1":
                S.op("dve", lambda E: E.tensor_copy(out=excl[:], in_=tots[:]), r=[r_f], w=[r_f])
            else:
                S.op("dve", lambda E: E.tensor_tensor_scan(out=excl[:], data0=onesr[:], data1=tots[:], initial=0.0, op0=ALU.mult, op1=ALU.add), r=[r_f], w=[r_f])
            S.op("dve", lambda E: E.tensor_tensor(out=excl[:], in0=excl[:], in1=tots[:], op=ALU.subtract), r=[r_f], w=[r_f])
            S.op("dve", lambda E: E.tensor_tensor(out=Cn[:], in0=pq[:, 0:NTL], in1=excl[:], op=ALU.add), r=[r_pq, r_f], w=[r_f])
            for kb in range(NTL):
                S.op("dve", lambda E, kb=kb: E.tensor_scalar(out=btab[:, kb, :], in0=excl[:], scalar1=Cn[:, kb:kb + 1], scalar2=-1.0, op0=ALU.subtract, op1=ALU.mult), r=[r_f], w=[r_in])

        def hf(gb0, Gc, Oi, r_Oi):
            S.op("dve", lambda E: E.reciprocal(out=rd[:, 0:Gc], in_=Oi[:, 0:Gc, 64]), r=[r_Oi], w=[r_h])
            for s in range(Gc):
                T = gb0 + s
                S.op("dve", lambda E, s=s, T=T: E.tensor_scalar(out=ybuf[:, T, 128:192], in0=Oi[:, s, 0:64], scalar1=rd[:, s:s + 1], scalar2=None, op0=ALU.mult), r=[r_Oi, r_h], w=[r_y[T]])

        if os.environ.get("FOXDBG") != "3":
          attention(fkT, fqT, 0, fva, 65, 4, cmaskb, None if os.environ.get("FOXDBG") == "2" else btab, [r_in], hf, psS, psO, pT, r_psS, r_psO, r_pT)
      S.barrier()

    if 3 in stages:
      with contextlib.ExitStack() as e3:
        sb3 = lambda name, shape, dt=F32: e3.enter_context(nc.sbuf_tensor("a3_" + name, shape, dt))
        ps3 = lambda name, shape, dt=F32: e3.enter_context(nc.psum_tensor("a3_" + name, shape, dt))
        Wc = [sb3("Wc%d" % j, [128, 8, 192], BF16) for j in range(4)]
        wcr = sb3("wcr", [128, 4, 192])
        sq = sb3("sq", [128, NTL, 192])
        gzz = sb3("gzz", [128, NTL, 66])
        ogd = sb3("ogd", [128, NTL, 64])
        xTc = [sb3("xTc%d" % i, [128, 8, 515], BF16) for i in range(2)]
        ssq = sb3("ssq", [128, NTL, 2])
        nbeta, bke, gg, gc, gtot, egc, ekd, ecd = [sb3(n, [128, NTL]) for n in ("nbeta", "bke", "gg", "gc", "gtot", "egc", "ekd", "ecd")]
        beta = sb3("beta", [128, NTL])
        gngb = sb3("gngb", [128, 64])
        kn, qn, kd = sb3("kn", [128, 64]), sb3("qn", [128, 64]), sb3("kd", [128, 64])
        knT, qnT, qdT, egr, wT = [sb3(n, [64, 128]) for n in ("knT", "qnT", "qdT", "egr", "wT")]
        gtri, Dm, DmT, Bm, BT, Bn, BTn, Rm, TTb, TTw, qkT = [sb3(n, [128, 128]) for n in ("gtri", "Dm", "DmT", "Bm", "BT", "Bn", "BTn", "Rm", "TTb", "TTw", "qkT")]
        u, vnew = sb3("u", [128, 64]), sb3("vnew", [128, 64])
        St = sb3("St", [64, 64])
        cmaskf = trif
        pc, pz, po = ps3("pc", [128, 512]), ps3("pz", [128, 512]), ps3("po", [128, 512])
        px = [ps3("px%d" % i, [128, 512]) for i in range(5)]
        r_px = [Res() for _ in range(5)]
        r_pc, r_pz, r_po = Res(), Res(), Res()
        r_Wc, r_sq, r_gzz, r_xc, r_b, r_S, r_og = Res(), [Res() for _ in range(NTL)], Res(), [Res(), Res()], Res(), Res(), [Res() for _ in range(NTL)]
        r_t = {n: Res() for n in ("kn", "qn", "kd", "knT", "qnT", "qdT", "egr", "wT", "gtri", "Dm", "DmT", "Bm", "BT", "Bn", "BTn", "Rm", "TTb", "TTw", "qkT", "u", "vnew")}
        ipx = [0]

        def nx():
            i = ipx[0] % 5
            ipx[0] += 1
            return px[i], r_px[i]

        S.dma("sp", wcr[:].rearrange("p a b -> p (a b)"), prm[:, P_GCW:P_GCW + 768].partition_broadcast(128), w=[r_Wc])
        S.dma("sp", gngb[:], prm[:, P_GNG:P_GNG + 64].partition_broadcast(128), w=[r_b])
        for j in range(4):
            for c in range(8):
                S.op("dve", lambda E, j=j, c=c: E.tensor_tensor(out=Wc[j][:, c, :], in0=wbf[:, c, C_GQ:C_GQ + 192], in1=wcr[:, j, :], op=ALU.mult), r=[r_w, r_Wc], w=[r_Wc])
        for ci, (t0, N) in enumerate(chunks):
            xc, r_x = xTc[ci % 2], r_xc[ci % 2]
            load_chunk(xc, r_x, t0, N, halo=3)
            for s in range(N // 128):
                T = t0 // 128 + s
                n = 0
                for j in range(4):
                    for c in range(8):
                        S.op("pe", lambda E, j=j, c=c, s=s, n=n: E.matmul(pc[:, 0:192], lhsT=xc[:, c, s * 128 + j:s * 128 + j + 128], rhs=Wc[j][:, c, :], start=(n == 0), stop=(n == 31)), r=[r_Wc, r_x], w=[r_pc])
                        n += 1
                S.op("act", lambda E, T=T: E.activation(out=sq[:, T, :], in_=pc[:, 0:192], func=AF.Silu), r=[r_pc], w=[r_sq[T]])
                for c in range(8):
                    S.op("pe", lambda E, c=c, s=s: E.matmul(pz[:, 0:66], lhsT=xc[:, c, 3 + s * 128:3 + (s + 1) * 128], rhs=wbf[:, c, C_GZ:C_GZ + 66], start=(c == 0), stop=(c == 7)), r=[r_w, r_x], w=[r_pz])
                S.op("dve", lambda E, T=T: E.tensor_copy(out=gzz[:, T, :], in_=pz[:, 0:66]), r=[r_pz], w=[r_gzz])
        for qk in range(2):
            S.op("dve", lambda E, qk=qk: E.tensor_tensor(out=ogd[:], in0=sq[:, :, qk * 64:(qk + 1) * 64], in1=sq[:, :, qk * 64:(qk + 1) * 64], op=ALU.mult), r=r_sq, w=[r_b])
            S.op("dve", lambda E, qk=qk: E.tensor_reduce(out=ssq[:, :, qk], in_=ogd[:], axis=AX.X, op=ALU.add), r=[r_b], w=[r_b])
        S.op("act", lambda E: E.activation(out=ssq[:], in_=ssq[:], func=AF.Sqrt, bias=c_eps6[:], scale=1.0), r=[r_b, r_c], w=[r_b])
        S.op("dve", lambda E: E.reciprocal(out=ssq[:], in_=ssq[:]), r=[r_b], w=[r_b])
        S.op("act", lambda E: E.activation(out=beta[:], in_=gzz[:, :, 64], func=AF.Sigmoid), r=[r_gzz], w=[r_b])
        S.op("dve", lambda E: E.tensor_scalar(out=nbeta[:], in0=beta[:], scalar1=-1.0, scalar2=None, op0=ALU.mult), r=[r_b], w=[r_b])
        S.op("act", lambda E: E.activation(out=gg[:], in_=gzz[:, :, 65], func=AF.Exp, bias=pcol[:, 1:2], scale=1.0), r=[r_gzz, r_pcol], w=[r_b])
        S.op("act", lambda E: E.activation(out=gg[:], in_=gg[:], func=AF.Ln, bias=c_one[:], scale=1.0), r=[r_b, r_c], w=[r_b])
        S.op("dve", lambda E: E.tensor_scalar(out=gg[:], in0=gg[:], scalar1=pcol[:, 2:3], scalar2=None, op0=ALU.mult), r=[r_b, r_pcol], w=[r_b])
        pa, r_pa = nx()
        S.op("pe", lambda E: E.matmul(pa[:, 0:NTL], lhsT=trif[:], rhs=gg[:], start=True, stop=True), r=[r_b, r_c], w=[r_pa])
        S.op("dve", lambda E: E.tensor_copy(out=gc[:], in_=pa[:, 0:NTL]), r=[r_pa], w=[r_b])
        pa2, r_pa2 = nx()
        S.op("pe", lambda E: E.matmul(pa2[:, 0:NTL], lhsT=onesf[:], rhs=gg[:], start=True, stop=True), r=[r_b, r_c], w=[r_pa2])
        S.op("dve", lambda E: E.tensor_copy(out=gtot[:], in_=pa2[:, 0:NTL]), r=[r_pa2], w=[r_b])
        S.op("act", lambda E: E.activation(out=egc[:], in_=gc[:], func=AF.Exp), r=[r_b], w=[r_b])
        S.op("act", lambda E: E.activation(out=ecd[:], in_=gtot[:], func=AF.Exp), r=[r_b], w=[r_b])
        S.op("dve", lambda E: E.tensor_tensor(out=ekd[:], in0=gtot[:], in1=gc[:], op=ALU.subtract), r=[r_b], w=[r_b])
        S.op("act", lambda E: E.activation(out=ekd[:], in_=ekd[:], func=AF.Exp), r=[r_b], w=[r_b])
        S.op("dve", lambda E: E.tensor_tensor(out=bke[:], in0=beta[:], in1=egc[:], op=ALU.mult), r=[r_b], w=[r_b])
        S.op("dve", lambda E: E.memset(St[:], 0.0), w=[r_S])

        def tr(dst, rdst, src, rsrc, npart, nfree):
            p, r_p = nx()
            S.op("pe", lambda E: E.transpose(out=p[0:nfree, 0:npart], in_=src, identity=identf[0:npart, 0:npart]), r=rsrc + [r_c], w=[r_p])
            S.op("act", lambda E: E.copy(out=dst, in_=p[0:nfree, 0:npart]), r=[r_p], w=[rdst])

        for T in range(NTL):
            rt = r_t
            S.op("dve", lambda E: E.tensor_scalar(out=kn[:], in0=sq[:, T, 64:128], scalar1=ssq[:, T, 1:2], scalar2=None, op0=ALU.mult), r=[r_sq[T], r_b], w=[rt["kn"]])
            S.op("dve", lambda E: E.tensor_scalar(out=qn[:], in0=sq[:, T, 0:64], scalar1=ssq[:, T, 0:1], scalar2=0.125, op0=ALU.mult, op1=ALU.mult), r=[r_sq[T], r_b], w=[rt["qn"]])
            tr(knT[:], rt["knT"], kn[:], [rt["kn"]], 128, 64)
            tr(qnT[:], rt["qnT"], qn[:], [rt["qn"]], 128, 64)
            S.op("dve", lambda E: E.tensor_scalar(out=gtri[:], in0=trif[:], scalar1=gg[:, T:T + 1], scalar2=None, op0=ALU.mult), r=[r_b, r_c], w=[rt["gtri"]])
            pg, r_pg = nx()
            S.op("pe", lambda E: E.matmul(pg[:, 0:128], lhsT=onesf[:], rhs=gtri[:], start=True, stop=True), r=[rt["gtri"], r_c], w=[r_pg])
            S.op("dve", lambda E: E.tensor_scalar(out=Dm[:], in0=pg[:, 0:128], scalar1=gc[:, T:T + 1], scalar2=0.0, op0=ALU.subtract, op1=ALU.max), r=[r_pg, r_b], w=[rt["Dm"]])
            S.op("act", lambda E: E.activation(out=Dm[:], in_=Dm[:], func=AF.Exp, scale=-1.0), r=[rt["Dm"]], w=[rt["Dm"]])
            S.op("dve", lambda E: E.tensor_scalar(out=DmT[:], in0=pg[:, 0:128], scalar1=gc[:, T:T + 1], scalar2=0.0, op0=ALU.subtract, op1=ALU.min), r=[r_pg, r_b], w=[rt["DmT"]])
            S.op("act", lambda E: E.activation(out=DmT[:], in_=DmT[:], func=AF.Exp), r=[rt["DmT"]], w=[rt["DmT"]])
            S.op("pool", lambda E: E.tensor_tensor(out=DmT[:], in0=DmT[:], in1=cmaskf[:], op=ALU.mult), r=[rt["DmT"], r_c], w=[rt["DmT"]])
            S.op("act", lambda E: E.activation(out=egr[:], in_=pg[0:64, 0:128], func=AF.Exp), r=[r_pg], w=[rt["egr"]])
            pkk, r_pkk = nx()
            S.op("pe", lambda E: E.matmul(pkk[:, 0:128], lhsT=knT[:], rhs=knT[:], start=True, stop=True), r=[rt["knT"]], w=[r_pkk])
            S.op("dve", lambda E: E.tensor_tensor(out=Bm[:], in0=pkk[:, 0:128], in1=Dm[:], op=ALU.mult), r=[r_pkk, rt["Dm"]], w=[rt["Bm"]])
            S.op("dve", lambda E: E.scalar_tensor_tensor(out=Bm[:], in0=Bm[:], scalar=nbeta[:, T:T + 1], in1=smask[:], op0=ALU.mult, op1=ALU.mult), r=[rt["Bm"], r_b, r_c], w=[rt["Bm"]])
            tr(BT[:], rt["BT"], Bm[:], [rt["Bm"]], 128, 128)
            S.op("pool", lambda E: E.tensor_tensor(out=Rm[:], in0=BT[:], in1=identf[:], op=ALU.add), r=[rt["BT"], r_c], w=[rt["Rm"]])
            cb, cbt, rcb, rcbt = Bm, BT, rt["Bm"], rt["BT"]
            nb_, nbt, rnb, rnbt = Bn, BTn, rt["Bn"], rt["BTn"]
            for lev in range(6):
                p1, r_p1 = nx()
                p2, r_p2 = nx()
                S.op("pe", lambda E: E.matmul(p1[:, 0:128], lhsT=cbt[:], rhs=cb[:], start=True, stop=True), r=[rcb, rcbt], w=[r_p1])
                S.op("pe", lambda E: E.matmul(p2[:, 0:128], lhsT=cb[:], rhs=cbt[:], start=True, stop=True), r=[rcb, rcbt], w=[r_p2])
                S.op("act", lambda E: E.copy(out=nb_[:], in_=p1[:, 0:128]), r=[r_p1], w=[rnb])
                S.op("dve", lambda E: E.tensor_copy(out=nbt[:], in_=p2[:, 0:128]), r=[r_p2], w=[rnbt])
                p3, r_p3 = nx()
                S.op("pe", lambda E: E.matmul(p3[:, 0:128], lhsT=nb_[:], rhs=Rm[:], start=True, stop=True), r=[rnb, rt["Rm"]], w=[r_p3])
                S.op("dve", lambda E: E.tensor_tensor(out=Rm[:], in0=Rm[:], in1=p3[:, 0:128], op=ALU.add), r=[r_p3], w=[rt["Rm"]])
                cb, cbt, rcb, rcbt, nb_, nbt, rnb, rnbt = nb_, nbt, rnb, rnbt, cb, cbt, rcb, rcbt
            S.op("dve", lambda E: E.tensor_scalar(out=TTb[:], in0=Rm[:], scalar1=beta[:, T:T + 1], scalar2=None, op0=ALU.mult), r=[rt["Rm"], r_b], w=[rt["TTb"]])
            S.op("pool", lambda E: E.tensor_scalar(out=TTw[:], in0=Rm[:], scalar1=bke[:, T:T + 1], scalar2=None, op0=ALU.mult), r=[rt["Rm"], r_b], w=[rt["TTw"]])
            pu, r_pu = nx()
            S.op("pe", lambda E: E.matmul(pu[:, 0:64], lhsT=TTb[:], rhs=sq[:, T, 128:192], start=True, stop=True), r=[rt["TTb"], r_sq[T]], w=[r_pu])
            S.op("act", lambda E: E.copy(out=u[:], in_=pu[:, 0:64]), r=[r_pu], w=[rt["u"]])
            pw, r_pw = nx()
            S.op("pe", lambda E: E.matmul(pw[0:64, 0:128], lhsT=kn[:], rhs=TTw[:], start=True, stop=True), r=[rt["kn"], rt["TTw"]], w=[r_pw])
            S.op("act", lambda E: E.copy(out=wT[:], in_=pw[0:64, 0:128]), r=[r_pw], w=[rt["wT"]])
            pqk, r_pqk = nx()
            S.op("pe", lambda E: E.matmul(pqk[:, 0:128], lhsT=knT[:], rhs=qnT[:], start=True, stop=True), r=[rt["knT"], rt["qnT"]], w=[r_pqk])
            S.op("dve", lambda E: E.tensor_tensor(out=qkT[:], in0=pqk[:, 0:128], in1=DmT[:], op=ALU.mult), r=[r_pqk, rt["DmT"]], w=[rt["qkT"]])
            S.op("dve", lambda E: E.tensor_tensor(out=qdT[:], in0=qnT[:], in1=egr[:], op=ALU.mult), r=[rt["qnT"], rt["egr"]], w=[rt["qdT"]])
            S.op("pool", lambda E: E.tensor_scalar(out=kd[:], in0=kn[:], scalar1=ekd[:, T:T + 1], scalar2=None, op0=ALU.mult), r=[rt["kn"], r_b], w=[rt["kd"]])
            pvn, r_pvn = nx()
            S.op("pe", lambda E: E.matmul(pvn[:, 0:64], lhsT=wT[:], rhs=St[:], start=True, stop=True), r=[rt["wT"], r_S], w=[r_pvn])
            S.op("dve", lambda E: E.tensor_tensor(out=vnew[:], in0=u[:], in1=pvn[:, 0:64], op=ALU.subtract), r=[rt["u"], r_pvn], w=[rt["vnew"]])
            S.op("pe", lambda E: E.matmul(po[:, 0:64], lhsT=qdT[:], rhs=St[:], start=True, stop=False), r=[rt["qdT"], r_S], w=[r_po])
            S.op("pe", lambda E: E.matmul(po[:, 0:64], lhsT=qkT[:], rhs=vnew[:], start=False, stop=True), r=[rt["qkT"], rt["vnew"]], w=[r_po])
            S.op("act", lambda E: E.copy(out=ogd[:, T, :], in_=po[:, 0:64]), r=[r_po, r_b], w=[r_og[T]])
            pS, r_pS = nx()
            S.op("pe", lambda E: E.matmul(pS[0:64, 0:64], lhsT=kd[:], rhs=vnew[:], start=True, stop=True), r=[rt["kd"], rt["vnew"]], w=[r_pS])
            S.op("dve", lambda E: E.scalar_tensor_tensor(out=St[:], in0=St[:], scalar=ecd[0:64, T:T + 1], in1=pS[0:64, 0:64], op0=ALU.mult, op1=ALU.add), r=[r_pS, r_b], w=[r_S])
        zs = sq
        S.op("act", lambda E: E.activation(out=zs[:, :, 0:64], in_=gzz[:, :, 0:64], func=AF.Silu), r=[r_gzz] + r_og, w=r_sq)
        S.op("dve", lambda E: E.tensor_tensor(out=zs[:, :, 64:128], in0=ogd[:], in1=ogd[:], op=ALU.mult), r=r_og, w=r_sq)
        S.op("dve", lambda E: E.tensor_reduce(out=ssq[:, :, 0], in_=zs[:, :, 64:128], axis=AX.X, op=ALU.add), r=r_sq, w=[r_b])
        S.op("act", lambda E: E.activation(out=ssq[:, :, 0], in_=ssq[:, :, 0], func=AF.Sqrt, bias=c_eps6[:], scale=1.0 / 64), r=[r_b, r_c], w=[r_b])
        S.op("dve", lambda E: E.reciprocal(out=ssq[:, :, 0], in_=ssq[:, :, 0]), r=[r_b], w=[r_b])
        for T in range(NTL):
            S.op("dve", lambda E, T=T: E.scalar_tensor_tensor(out=kn[:], in0=ogd[:, T, :], scalar=ssq[:, T, 0:1], in1=gngb[:], op0=ALU.mult, op1=ALU.mult), r=[r_og[T], r_b], w=[r_t["kn"]])
            S.op("dve", lambda E, T=T: E.tensor_tensor(out=ybuf[:, T, 192:256], in0=kn[:], in1=zs[:, T, 0:64], op=ALU.mult), r=[r_t["kn"]] + r_sq[T:T + 1], w=[r_y[T]])
      S.barrier()

    S.op("pool", lambda E: E.memset(ybuf[0:112, 0, :], 0.0), r=[r_y[0]], w=[r_y[0]])
    for T in range(NTL):
        S.dma("sp", y[T * 128:(T + 1) * 128, :], ybuf[:, T, :], r=[r_y[T]], is_out=True)
    S.finish()
    return nc


def _lam_init(layer):
    import math
    return 0.8 - 0.6 * math.exp(-0.3 * layer)


def _prep_A_weights(inp, layer, j):
    w_in = inp["w_in"][layer]
    wc = np.zeros((D, WC), np.float32)
    wc[:, C_DQ:C_DQ + 128] = w_in[:, j * 128:(j + 1) * 128]
    wc[:, C_DK:C_DK + 128] = w_in[:, 512 + j * 128:512 + (j + 1) * 128]
    wc[:, C_DV:C_DV + 128] = w_in[:, 1024 + j * 128:1024 + (j + 1) * 128]
    wc[:, C_FQ:C_FQ + 64] = w_in[:, 1536 + j * 64:1536 + (j + 1) * 64]
    wc[:, C_FK:C_FK + 64] = w_in[:, 1792 + j * 64:1792 + (j + 1) * 64]
    wc[:, C_FK + 64:C_FK + 128] = w_in[:, 1536 + j * 64:1536 + (j + 1) * 64]
    wc[:, C_FV:C_FV + 64] = w_in[:, 2048 + j * 64:2048 + (j + 1) * 64]
    wc[:, C_FV + 64] = w_in[:, 2304 + j]
    wc[:, C_GQ:C_GQ + 64] = w_in[:, 2308 + j * 64:2308 + (j + 1) * 64]
    wc[:, C_GQ + 64:C_GQ + 128] = w_in[:, 2564 + j * 64:2564 + (j + 1) * 64]
    wc[:, C_GQ + 128:C_GQ + 192] = w_in[:, 2820 + j * 64:2820 + (j + 1) * 64]
    wc[:, C_GZ:C_GZ + 64] = w_in[:, 3084 + j * 64:3084 + (j + 1) * 64]
    wc[:, C_GZ + 64] = w_in[:, 3076 + j]
    wc[:, C_GZ + 65] = w_in[:, 3080 + j]
    prm = np.zeros((1, PRM), np.float32)
    prm[0, P_LAM:P_LAM + 256] = inp["diff_lambda"][layer].reshape(-1)
    prm[0, P_DSG:P_DSG + 128] = inp["diff_subln_g"][layer]
    prm[0, P_FFB] = inp["fox_forget_b"][layer][j]
    prm[0, P_ALOG] = inp["gdn_a_log"][layer][j]
    prm[0, P_DTB] = inp["gdn_dt_bias"][layer][j]
    prm[0, P_GNG:P_GNG + 64] = inp["gdn_norm_g"][layer]
    cw = inp["gdn_conv_w"][layer]
    g = np.concatenate([cw[:, j * 64:(j + 1) * 64], cw[:, 256 + j * 64:256 + (j + 1) * 64], cw[:, 512 + j * 64:512 + (j + 1) * 64]], axis=1)
    prm[0, P_GCW:P_GCW + 768] = g.reshape(-1)
    return wc, prm


def _tiles(arr, r):
    a = arr[TILE_START[r] * 128:TILE_START[r + 1] * 128]
    pad = NTB * 128 - a.shape[0]
    if pad:
        a = np.concatenate([a, a[:pad]], 0)
    return np.ascontiguousarray(a)


def _untile(parts):
    return np.concatenate([parts[r][:(TILE_START[r + 1] - TILE_START[r]) * 128] for r in range(4)], 0)


def kernel(**inp):
    inp = {k: np.asarray(v) for k, v in inp.items()}
    cores = list(range(8))
    nc_B = build_B()
    stream32 = None
    streambf = None
    for layer in range(2):
        nc_A = build_A(layer == 0, _lam_init(layer))
        in_maps = []
        for c in cores:
            b, j = divmod(c, 4)
            wc, prm = _prep_A_weights(inp, layer, j)
            m = {"w": wc, "prm": prm}
            if layer == 0:
                m.update({"x": np.ascontiguousarray(inp["x"][b]), "meta": inp["meta_tokens"],
                          "lng": inp["ln_in_g"][None], "lnb": inp["ln_in_b"][None]})
            else:
                m["sbf"] = streambf[b]
            in_maps.append(m)
        resA = run_bass_kernel_spmd(nc_A, in_maps, core_ids=cores).results
        if layer == 0:
            stream32 = [resA[0]["s32o"], resA[4]["s32o"]]
        yfull = []
        for b in range(2):
            yb = np.zeros((L, D), ml_dtypes.bfloat16)
            for j in range(4):
                yc = resA[4 * b + j]["y"]
                yb[:, j * 128:(j + 1) * 128] = yc[:, 0:128]
                yb[:, 512 + j * 64:512 + (j + 1) * 64] = yc[:, 128:192]
                yb[:, 768 + j * 64:768 + (j + 1) * 64] = yc[:, 192:256]
            yfull.append(yb)
        in_maps = []
        for c in cores:
            b, r = divmod(c, 4)
            in_maps.append({"yg": _tiles(yfull[b], r), "s32": _tiles(stream32[b], r),
                            "wout": inp["w_out"][layer], "ln1g": inp["ln1_g"][layer][None], "ln1b": inp["ln1_b"][layer][None],
                            "ln2g": inp["ln2_g"][layer][None], "ln2b": inp["ln2_b"][layer][None],
                            "rw": inp["router_w"][layer], "rb": inp["router_b"][layer][None],
                            "w1": inp["expert_w1"][layer], "b1": inp["expert_b1"][layer],
                            "w2": inp["expert_w2"][layer], "b2": inp["expert_b2"][layer]})
        resB = run_bass_kernel_spmd(nc_B, in_maps, core_ids=cores).results
        stream32 = [_untile([resB[4 * b + r]["out32"] for r in range(4)]) for b in range(2)]
        streambf = [_untile([resB[4 * b + r]["outbf"] for r in range(4)]) for b in range(2)]
    out = np.stack([stream32[b][128:] for b in range(2)], 0).astype(np.float32)
    return out
```
